# Optimizing a Trainium2 kernel written in Bass

```python
import math
import jax
import jax.numpy as jnp
from jax import lax
import numpy as np

D_MODEL = 1024
BATCH = 4
SEQ = 8192
DEPTH = 1

GRID_W = 64
CTX_LEN = 256
N_MOD = 6
EPS = 1e-6

SSM_WIDTH = D_MODEL // 2
SSM_GROUP = 16
SSM_GROUPS = SSM_WIDTH // SSM_GROUP
SSM_STATE = 64
STEP_MIN = 1e-3
STEP_MAX = 1e-1

HEAD_DIM = 64
N_HEADS = (D_MODEL // 2) // HEAD_DIM
N_KV_HEADS = N_HEADS // 4
GQA_GROUP = N_HEADS // N_KV_HEADS
ATTN_WIDTH = N_HEADS * HEAD_DIM
KV_WIDTH = N_KV_HEADS * HEAD_DIM
ROPE_THETA = 10000.0
Q_BLOCK = 128

COL_K = SSM_WIDTH
COL_V = COL_K + KV_WIDTH
COL_Q = COL_V + KV_WIDTH
COL_GATE = COL_Q + ATTN_WIDTH
D_IN = COL_GATE + 2 * D_MODEL

N_EXPERTS = 32
TOP_K = 4
D_EXPERT = D_MODEL
SWIGLU_ALPHA = 1.702
SWIGLU_LIMIT = 7.0
MOE_BLOCK = 256

kernel_name = 'hybrid_s5_gqa_moe_dit_block'


def _silu(x):
    return x * jax.nn.sigmoid(x)


def _rmsnorm(x, g):
    xf = x.astype(jnp.float32)
    y = xf * lax.rsqrt(jnp.mean(xf * xf, axis=-1, keepdims=True) + EPS)
    return (y * g.astype(jnp.float32)).astype(x.dtype)


def _modulate(h, shift, scale):
    return h * (1 + scale) + shift


def _scan_combine(left, right):
    a_l, b_l = left
    a_r, b_r = right
    return a_r * a_l, a_r * b_l + b_r


def _diag_scan(lbar, bu, h0, reverse):
    if h0 is not None:
        edge = -1 if reverse else 0
        bu = bu.at[:, edge].add(lbar * h0)
    a = jnp.broadcast_to(lbar, (1,) + bu.shape[1:])
    _, h = lax.associative_scan(_scan_combine, (a, bu), reverse=reverse, axis=1)
    return h


def _s5_discretise(lam_re, lam_im, log_step, b_re, b_im):
    f32 = jnp.float32
    lam = lax.complex(lam_re.astype(f32), lam_im.astype(f32))
    step = jnp.exp(log_step.astype(f32))[:, None]
    lbar = jnp.exp(lam * step)
    bmat = lax.complex(b_re.astype(f32), b_im.astype(f32))
    bbar = ((lbar - 1) / lam)[..., None] * bmat
    return lbar, bbar


def _s5_readout(h, cmat):
    b_, l_ = h.shape[:2]
    return jnp.einsum('blgn,gpn->blgp', h, cmat).real.reshape(b_, l_, SSM_WIDTH)


def _s5_mixer(u, u_c, lam_re, lam_im, log_step, b_re, b_im, c_re, c_im, d_skip, with_ctx_out):
    f32 = jnp.float32
    b_, l_, _ = u.shape
    lc = u_c.shape[1]
    uf = u.astype(f32)
    ucf = u_c.astype(f32)
    ug = uf.reshape(b_, l_, SSM_GROUPS, SSM_GROUP)
    ucg = ucf.reshape(b_, lc, SSM_GROUPS, SSM_GROUP)
    d = d_skip.astype(f32)
    y = uf * d
    y_c = ucf * d if with_ctx_out else None
    for direction, reverse in enumerate((False, True)):
        lbar, bbar = _s5_discretise(lam_re[direction], lam_im[direction], log_step[direction],
                                    b_re[direction], b_im[direction])
        cmat = lax.complex(c_re[direction].astype(f32), c_im[direction].astype(f32))
        h_c = _diag_scan(lbar, jnp.einsum('blgp,gnp->blgn', ucg, bbar), None, reverse)
        h0 = h_c[:, 0] if reverse else h_c[:, -1]
        h = _diag_scan(lbar, jnp.einsum('blgp,gnp->blgn', ug, bbar), h0, reverse)
        y = y + _s5_readout(h, cmat)
        if with_ctx_out:
            y_c = y_c + _s5_readout(h_c, cmat)
    return y.astype(u.dtype), (y_c.astype(u.dtype) if with_ctx_out else None)


def _axial_rope_tables(row_id, col_id):
    half = HEAD_DIM // 2
    inv_freq = ROPE_THETA ** (-jnp.arange(0, half, 2, dtype=jnp.float32) / half)
    ang = jnp.concatenate([row_id.astype(jnp.float32)[:, None] * inv_freq,
                           col_id.astype(jnp.float32)[:, None] * inv_freq], axis=-1)
    return jnp.cos(ang), jnp.sin(ang)


def _rope(x, cos, sin):
    xf = x.astype(jnp.float32)
    x1, x2 = xf[..., 0::2], xf[..., 1::2]
    cos = cos[None, :, None, :]
    sin = sin[None, :, None, :]
    out = jnp.stack([x1 * cos - x2 * sin, x1 * sin + x2 * cos], axis=-1).reshape(x.shape)
    return out.astype(x.dtype)


def _attention(q, k, v):
    b_, lq = q.shape[:2]
    nb = lq // Q_BLOCK
    scale = HEAD_DIM ** -0.5
    qb = q.reshape(b_, nb, Q_BLOCK, N_KV_HEADS, GQA_GROUP, HEAD_DIM).transpose(1, 0, 2, 3, 4, 5)

    def one_block(qblk):
        s = jnp.einsum('bqkgd,bskd->bkgqs', qblk, k).astype(jnp.float32) * scale
        p = jax.nn.softmax(s, axis=-1).astype(v.dtype)
        return jnp.einsum('bkgqs,bskd->bqkgd', p, v)

    o = lax.map(one_block, qb)
    return o.transpose(1, 0, 2, 3, 4, 5).reshape(b_, lq, ATTN_WIDTH)


def _merge_branches(y_ssm, o_attn, gate_logits, w_glu, b_glu, w_ssm_out, w_attn_out, w_out):
    s = jax.nn.gelu(y_ssm)
    s = s * jax.nn.sigmoid(s @ w_glu + b_glu)
    gates = jax.nn.sigmoid(gate_logits)
    merged = gates[..., :D_MODEL] * (s @ w_ssm_out) + gates[..., D_MODEL:] * (o_attn @ w_attn_out)
    return merged @ w_out


def _moe(h, router_w, router_b, w_gate_up, b_gate_up, w_down, b_down):
    t_ = h.shape[0]
    m_ = t_ * TOP_K
    nb = (m_ + N_EXPERTS * (MOE_BLOCK - 1)) // MOE_BLOCK
    logits = (h @ router_w + router_b).astype(jnp.float32)
    top_vals, top_idx = lax.top_k(logits, TOP_K)
    gates = jax.nn.softmax(top_vals, axis=-1)
    flat_e = top_idx.reshape(-1)
    order = jnp.argsort(flat_e)
    sorted_e = flat_e[order]
    sorted_tok = order // TOP_K
    counts = jnp.bincount(flat_e, length=N_EXPERTS)
    starts = jnp.cumsum(counts) - counts
    blk_counts = (counts + MOE_BLOCK - 1) // MOE_BLOCK
    blk_ends = jnp.cumsum(blk_counts)
    pad_starts = (blk_ends - blk_counts) * MOE_BLOCK
    dest = pad_starts[sorted_e] + (jnp.arange(m_) - starts[sorted_e])
    n_slots = nb * MOE_BLOCK
    slot_tok = jnp.full((n_slots,), t_, jnp.int32).at[dest].set(sorted_tok.astype(jnp.int32))
    slot_gate = jnp.zeros((n_slots,), jnp.float32).at[dest].set(gates.reshape(-1)[order])
    block_expert = jnp.minimum(jnp.searchsorted(blk_ends, jnp.arange(nb), side='right'), N_EXPERTS - 1)
    h_pad = jnp.concatenate([h, jnp.zeros((1, h.shape[1]), h.dtype)], axis=0)
    xb = h_pad[slot_tok].reshape(nb, MOE_BLOCK, h.shape[1])

    def expert_block(args):
        xblk, e = args
        gu = xblk @ w_gate_up[e] + b_gate_up[e]
        gate, up = gu[..., :D_EXPERT], gu[..., D_EXPERT:]
        gate = jnp.minimum(gate, SWIGLU_LIMIT)
        up = jnp.clip(up, -SWIGLU_LIMIT, SWIGLU_LIMIT)
        act = (up + 1) * (gate * jax.nn.sigmoid(SWIGLU_ALPHA * gate))
        return act @ w_down[e] + b_down[e]

    yb = lax.map(expert_block, (xb, block_expert)).reshape(n_slots, -1).astype(jnp.float32)
    out = jnp.zeros((t_ + 1, h.shape[1]), jnp.float32).at[slot_tok].add(yb * slot_gate[:, None])
    return out[:t_].astype(h.dtype)


def setup_inputs(seed: int = 0) -> dict:
    key = jax.random.key(seed)
    ks = jax.random.split(key, 40)
    f32 = jnp.float32

    def nrm(k, shape, scale):
        return jax.random.normal(k, shape, f32) * scale

    def gain(k, shape):
        return 1.0 + 0.05 * jax.random.normal(k, shape, f32)

    n_idx = jnp.arange(SSM_STATE, dtype=f32)
    lam_shape = (DEPTH, 2, SSM_GROUPS, SSM_STATE)
    return {
        'x': nrm(ks[0], (BATCH, SEQ, D_MODEL), 1.0),
        'c': nrm(ks[1], (BATCH, D_MODEL), 1.0),
        'ctx': nrm(ks[2], (BATCH, CTX_LEN, D_MODEL), 1.0),
        'c_ctx': nrm(ks[3], (D_MODEL,), 1.0),
        'w_mod': nrm(ks[4], (DEPTH, D_MODEL, N_MOD * D_MODEL), 0.5 * D_MODEL ** -0.5),
        'b_mod': nrm(ks[5], (DEPTH, N_MOD * D_MODEL), 0.02),
        'norm1_g': gain(ks[6], (DEPTH, D_MODEL)),
        'norm2_g': gain(ks[7], (DEPTH, D_MODEL)),
        'w_in': nrm(ks[8], (DEPTH, D_MODEL, D_IN), D_MODEL ** -0.5),
        's5_lam_re': -0.5 + nrm(ks[9], lam_shape, 0.01),
        's5_lam_im': math.pi * n_idx + nrm(ks[10], lam_shape, 0.01),
        's5_log_step': jax.random.uniform(ks[11], (DEPTH, 2, SSM_GROUPS), f32,
                                          minval=math.log(STEP_MIN), maxval=math.log(STEP_MAX)),
        's5_b_re': nrm(ks[12], (DEPTH, 2, SSM_GROUPS, SSM_STATE, SSM_GROUP), (2 * SSM_GROUP) ** -0.5),
        's5_b_im': nrm(ks[13], (DEPTH, 2, SSM_GROUPS, SSM_STATE, SSM_GROUP), (2 * SSM_GROUP) ** -0.5),
        's5_c_re': nrm(ks[14], (DEPTH, 2, SSM_GROUPS, SSM_GROUP, SSM_STATE), SSM_STATE ** -0.5),
        's5_c_im': nrm(ks[15], (DEPTH, 2, SSM_GROUPS, SSM_GROUP, SSM_STATE), SSM_STATE ** -0.5),
        's5_d': nrm(ks[16], (DEPTH, SSM_WIDTH), 1.0),
        'w_glu': nrm(ks[17], (DEPTH, SSM_WIDTH, SSM_WIDTH), SSM_WIDTH ** -0.5),
        'b_glu': nrm(ks[18], (DEPTH, SSM_WIDTH), 0.02),
        'w_ssm_out': nrm(ks[19], (DEPTH, SSM_WIDTH, D_MODEL), SSM_WIDTH ** -0.5),
        'q_norm_g': gain(ks[20], (DEPTH, HEAD_DIM)),
        'k_norm_g': gain(ks[21], (DEPTH, HEAD_DIM)),
        'w_attn_out': nrm(ks[22], (DEPTH, ATTN_WIDTH, D_MODEL), ATTN_WIDTH ** -0.5),
        'w_out': nrm(ks[23], (DEPTH, D_MODEL, D_MODEL), D_MODEL ** -0.5),
        'router_w': nrm(ks[24], (DEPTH, D_MODEL, N_EXPERTS), D_MODEL ** -0.5),
        'router_b': nrm(ks[25], (DEPTH, N_EXPERTS), 0.01),
        'w_gate_up': nrm(ks[26], (DEPTH, N_EXPERTS, D_MODEL, 2 * D_EXPERT), D_MODEL ** -0.5),
        'b_gate_up': nrm(ks[27], (DEPTH, N_EXPERTS, 2 * D_EXPERT), 0.02),
        'w_down': nrm(ks[28], (DEPTH, N_EXPERTS, D_EXPERT, D_MODEL), D_EXPERT ** -0.5),
        'b_down': nrm(ks[29], (DEPTH, N_EXPERTS, D_MODEL), 0.02),
        'final_norm_g': gain(ks[30], (D_MODEL,)),
    }


def reference(x, c, ctx, c_ctx, w_mod, b_mod, norm1_g, norm2_g, w_in,
              s5_lam_re, s5_lam_im, s5_log_step, s5_b_re, s5_b_im, s5_c_re, s5_c_im, s5_d,
              w_glu, b_glu, w_ssm_out, q_norm_g, k_norm_g, w_attn_out, w_out,
              router_w, router_b, w_gate_up, b_gate_up, w_down, b_down, final_norm_g):
    b_, l_, _ = x.shape
    lc = ctx.shape[1]
    rows = l_ // GRID_W
    row_id = jnp.repeat(jnp.arange(rows, dtype=jnp.int32), GRID_W)
    col_id = jnp.arange(rows * GRID_W, dtype=jnp.int32) % GRID_W
    cos, sin = _axial_rope_tables(row_id, col_id)

    for i in range(DEPTH):
        update_ctx = i < DEPTH - 1
        mod = (_silu(c) @ w_mod[i] + b_mod[i])[:, None, :]
        sh1, sc1, g1, sh2, sc2, g2 = jnp.split(mod, N_MOD, axis=-1)
        mod_c = _silu(c_ctx) @ w_mod[i] + b_mod[i]
        sh1c, sc1c, g1c, sh2c, sc2c, g2c = jnp.split(mod_c, N_MOD, axis=-1)

        h = _modulate(_rmsnorm(x, norm1_g[i]), sh1, sc1)
        hc = _modulate(_rmsnorm(ctx, norm1_g[i]), sh1c, sc1c)
        proj = h @ w_in[i]
        n_ctx_cols = D_IN if update_ctx else COL_Q
        proj_c = hc @ w_in[i][:, :n_ctx_cols]

        y_ssm, y_ssm_c = _s5_mixer(proj[..., :SSM_WIDTH], proj_c[..., :SSM_WIDTH],
                                   s5_lam_re[i], s5_lam_im[i], s5_log_step[i],
                                   s5_b_re[i], s5_b_im[i], s5_c_re[i], s5_c_im[i], s5_d[i],
                                   update_ctx)

        q = proj[..., COL_Q:COL_GATE].reshape(b_, l_, N_HEADS, HEAD_DIM)
        k = proj[..., COL_K:COL_V].reshape(b_, l_, N_KV_HEADS, HEAD_DIM)
        v = proj[..., COL_V:COL_Q].reshape(b_, l_, N_KV_HEADS, HEAD_DIM)
        q = _rope(_rmsnorm(q, q_norm_g[i]), cos, sin)
        k = _rope(_rmsnorm(k, k_norm_g[i]), cos, sin)
        k_c = _rmsnorm(proj_c[..., COL_K:COL_V].reshape(b_, lc, N_KV_HEADS, HEAD_DIM), k_norm_g[i])
        v_c = proj_c[..., COL_V:COL_Q].reshape(b_, lc, N_KV_HEADS, HEAD_DIM)
        k_all = jnp.concatenate([k_c, k], axis=1)
        v_all = jnp.concatenate([v_c, v], axis=1)
        o_attn = _attention(q, k_all, v_all)

        x_mix = x + g1 * _merge_branches(y_ssm, o_attn, proj[..., COL_GATE:],
                                         w_glu[i], b_glu[i], w_ssm_out[i], w_attn_out[i], w_out[i])

        h2 = _modulate(_rmsnorm(x_mix, norm2_g[i]), sh2, sc2).reshape(b_ * l_, D_MODEL)
        if update_ctx:
            q_c = _rmsnorm(proj_c[..., COL_Q:COL_GATE].reshape(b_, lc, N_HEADS, HEAD_DIM), q_norm_g[i])
            o_attn_c = _attention(q_c, k_c, v_c)
            ctx_mix = ctx + g1c * _merge_branches(y_ssm_c, o_attn_c, proj_c[..., COL_GATE:],
                                                  w_glu[i], b_glu[i], w_ssm_out[i], w_attn_out[i], w_out[i])
            h2c = _modulate(_rmsnorm(ctx_mix, norm2_g[i]), sh2c, sc2c).reshape(b_ * lc, D_MODEL)
            f = _moe(jnp.concatenate([h2, h2c], axis=0), router_w[i], router_b[i],
                     w_gate_up[i], b_gate_up[i], w_down[i], b_down[i])
            x = x_mix + g2 * f[:b_ * l_].reshape(x.shape)
            ctx = ctx_mix + g2c * f[b_ * l_:].reshape(ctx.shape)
        else:
            f = _moe(h2, router_w[i], router_b[i], w_gate_up[i], b_gate_up[i], w_down[i], b_down[i])
            x = x_mix + g2 * f.reshape(x.shape)

    return _rmsnorm(x, final_norm_g)
```

```python
import os
import numpy as np
from contextlib import ExitStack
import concourse.bass as bass
import concourse.mybir as mybir
from concourse.bass_utils import run_bass_kernel_spmd

F32 = mybir.dt.float32
BF16 = mybir.dt.bfloat16
AF = mybir.ActivationFunctionType
ALU = mybir.AluOpType
AX = mybir.AxisListType

ENGS = ("pe", "act", "dve", "pool", "sp")
D = 1024
L = 8192
LC = 256
NOWN = 4096
NALL = LC + L
DIN = 3328
EPS = 1e-6


_ALL_BUFS = []


class Buf:
    def __init__(self, name, h):
        _ALL_BUFS.append(self)
        self.name = name
        self.h = h
        self.last_w = None
        self.readers = []
        self.dma_sem = None
        self.dma_cnt = 0

    def __getitem__(self, idx):
        return self.h[idx]


class Sched:
    def __init__(self, nc, stack):
        self.nc = nc
        self.stack = stack
        self.sems = {}
        self.free_sems = []
        self.used_sems = []
        self.nsem = 0
        self.epoch = {e: 0 for e in ENGS}
        for e in ENGS:
            self.sems[(e, 0)] = self._new_sem()
        self.cnt = {e: 0 for e in ENGS}
        self.prog = {e: [] for e in ENGS}
        self.waited = {e: {} for e in ENGS}
        self.nbuf = 0
        self.sb = {}
        self.dma_keys = {}
        self.ph = None

    def _new_sem(self):
        if self.free_sems:
            sm = self.free_sems.pop()
        else:
            self.nsem += 1
            sm = self.stack.enter_context(self.nc.semaphore(f"sem{self.nsem}"))
        self.used_sems.append(sm)
        return sm

    def phase(self):
        self.ph = ExitStack()
        return self.ph

    SB_LIMIT = 172 * 1024

    def sbuf(self, name, shape, dt):
        nb = int(np.prod(shape[1:])) * (4 if dt == F32 else 2)
        nb = (nb + 31) // 32 * 32
        key = "p" if self.ph is self.stack else "t"
        self.sb[key] = self.sb.get(key, 0) + nb
        assert self.sb.get("p", 0) + self.sb.get("t", 0) <= self.SB_LIMIT, (name, self.sb)
        self.nbuf += 1
        h = self.ph.enter_context(self.nc.sbuf_tensor(f"{name}_{self.nbuf}", list(shape), dt))
        return Buf(name, h)

    def psum(self, name, shape, dt):
        self.nbuf += 1
        h = self.ph.enter_context(self.nc.psum_tensor(f"{name}_{self.nbuf}", list(shape), dt))
        return Buf(name, h)

    def dram(self, name, shape, dt, kind="Internal"):
        t = self.nc.dram_tensor(name, list(shape), dt, kind=kind)
        return Buf(name, t.ap())

    def _dma_sem(self, buf):
        if buf.dma_sem is None:
            self.nbuf += 1
            buf.dma_sem = self._new_sem()
            buf.dma_key = ("d", self.nbuf)
            buf.dma_cnt = 0
            self.sems[buf.dma_key] = buf.dma_sem
        return buf.dma_sem

    def _collect(self, eng, reads, writes):
        need = {}

        def add(ev):
            if ev is None:
                return
            k, v = ev
            if need.get(k, 0) < v:
                need[k] = v
        for b in reads:
            add(b.last_w)
        for b in writes:
            add(b.last_w)
            for ev in b.readers:
                add(ev)
        waits = []
        for k, v in need.items():
            if self.waited[eng].get(k, 0) >= v:
                continue
            self.waited[eng][k] = v
            waits.append((k, v))
        return waits

    def _commit(self, ev, reads, writes):
        for b in writes:
            b.last_w = ev
            b.readers = []
        for b in reads:
            if b not in writes:
                b.readers.append(ev)
                if len(b.readers) > 16:
                    best = {}
                    for k, v in b.readers:
                        if best.get(k, 0) < v:
                            best[k] = v
                    b.readers = list(best.items())

    LIMIT = 3000

    def _roll(self, eng):
        if self.cnt[eng] >= self.EPOCH:
            self.epoch[eng] += 1
            self.cnt[eng] = 0
            k = (eng, self.epoch[eng])
            self.sems[k] = self._new_sem()

    def op(self, eng, fn, reads=(), writes=()):
        assert self.cnt[eng] < 4000, "semaphore count too large: add maybe_sync()"
        waits = self._collect(eng, reads, writes)
        self.cnt[eng] += 1
        key = (eng, self.epoch[eng])
        ev = (key, self.cnt[eng])
        self.prog[eng].append((waits, fn, (key, 1)))
        self._commit(ev, reads, writes)
        return ev

    def dma(self, q, fns, sem_buf, reads=(), writes=()):
        assert sem_buf.dma_cnt < 4000, "dma semaphore count too large: add maybe_sync()"
        self._dma_sem(sem_buf)
        key = sem_buf.dma_key
        waits = self._collect(q, reads, writes)
        for i, fn in enumerate(fns):
            sem_buf.dma_cnt += 16
            self.prog[q].append((waits if i == 0 else [], fn, (key, 16)))
        ev = (key, sem_buf.dma_cnt)
        self.dma_keys[key] = sem_buf.dma_cnt
        self._commit(ev, reads, writes)
        return ev

    def barrier(self):
        evs = [((e, self.epoch[e]), self.cnt[e]) for e in ENGS if self.cnt[e] > 0]
        evs += list(self.dma_keys.items())
        for e in ENGS:
            waits = []
            for k, v in evs:
                if k == (e, self.epoch[e]) or self.waited[e].get(k, 0) >= v:
                    continue
                self.waited[e][k] = v
                waits.append((k, v))
            if waits:
                self.prog[e].append((waits, None, None))

    def emit(self):
        nc = self.nc
        with nc.Block() as block:
            deco = {"pe": block.tensor, "act": block.scalar, "dve": block.vector,
                    "pool": block.gpsimd, "sp": block.sync}
            for e in ENGS:
                prog = self.prog[e]
                if not prog:
                    continue

                def body(engine, prog=prog):
                    for waits, fn, inc in prog:
                        for k, v in waits:
                            engine.wait_ge(self.sems[k], v)
                        if fn is None:
                            continue
                        fn(engine).then_inc(self.sems[inc[0]], inc[1])

                deco[e](body)
        self.prog = {e: [] for e in ENGS}

    def sync_all(self):
        if os.environ.get("SYNCDBG"):
            print("SYNC", getattr(self, "nsync", 0), dict(self.cnt), {e: len(self.prog[e]) for e in ENGS}, flush=True)
        self.barrier()
        if not hasattr(self, "hs_pairs"):
            self.hs_pairs = [(self.stack.enter_context(self.nc.semaphore(f"hs{i}")), self.stack.enter_context(self.nc.semaphore(f"go{i}"))) for i in range(2)]
            self.nsync = 0
        pair = self.hs_pairs[self.nsync % 2]
        other = self.hs_pairs[(self.nsync + 1) % 2]
        self.nsync += 1
        used = list(self.used_sems) + list(other)
        for e in ENGS:
            if e == "sp":
                continue
            self.prog[e].append(([], lambda en: en.nop(), ("__hs", 1)))
            self.prog[e].append(([("__go", 1)], None, None))
        self.sems["__hs"], self.sems["__go"] = pair

        def clr(en):
            for sm in used:
                en.sem_clear(sm)
            return en.nop()
        self.prog["sp"].append(([("__hs", 4)], clr, ("__go", 1)))
        self.emit()
        self.free_sems.extend(self.used_sems)
        self.used_sems = []
        self.sems = {}
        self.dma_keys = {}
        self.waited = {e: {} for e in ENGS}
        for e in ENGS:
            self.epoch[e] += 1
            self.cnt[e] = 0
            self.sems[(e, self.epoch[e])] = self._new_sem()
        for b in _ALL_BUFS:
            b.last_w = None
            b.readers = []
            b.dma_sem = None
            b.dma_cnt = 0

    def maybe_sync(self, lim=1500):
        m = max(max(self.cnt.values()), max([b.dma_cnt for b in _ALL_BUFS] + [0]))
        if m >= lim:
            self.sync_all()

    def end_phase(self):
        self.sync_all()
        self.ph.close()
        self.ph = None
        self.sb["t"] = 0

    def mm(self, out, lhsT, rhs, start, stop, R, W):
        return self.op("pe", lambda e: e.matmul(out, lhsT, rhs, start=start, stop=stop), R, W)

    def tr(self, out, in_, ident, R, W):
        return self.op("pe", lambda e: e.transpose(out, in_, ident), R, W)

    def act(self, out, in_, func, R, W, bias=None, scale=None, accum=None):
        kw = {}
        if bias is not None:
            kw["bias"] = bias
        if scale is not None:
            kw["scale"] = scale
        if accum is not None:
            kw["accum_out"] = accum
        return self.op("act", lambda e: e.activation(out=out, in_=in_, func=func, **kw), R, W)

    def tt(self, eng, out, a, b, op, R, W):
        return self.op(eng, lambda e: e.tensor_tensor(out=out, in0=a, in1=b, op=op), R, W)

    def ts(self, eng, out, a, s1, s2, op0, op1, R, W):
        if op1 is None:
            return self.op(eng, lambda e: e.tensor_scalar(out=out, in0=a, scalar1=s1, scalar2=None, op0=op0), R, W)
        return self.op(eng, lambda e: e.tensor_scalar(out=out, in0=a, scalar1=s1, scalar2=s2, op0=op0, op1=op1), R, W)

    def stt(self, out, a, s, b, op0, op1, R, W):
        return self.op("dve", lambda e: e.scalar_tensor_tensor(out=out, in0=a, scalar=s, in1=b, op0=op0, op1=op1), R, W)

    def cp(self, eng, out, in_, R, W):
        if eng == "act" or (out.dtype == in_.dtype and out.dtype == BF16):
            return self.op("act", lambda e: e.activation(out=out, in_=in_, func=AF.Identity), R, W)
        if out.dtype == in_.dtype:
            return self.op(eng, lambda e: e.tensor_scalar(out=out, in0=in_, scalar1=1.0, scalar2=None, op0=ALU.mult), R, W)
        return self.op(eng, lambda e: e.tensor_copy(out=out, in_=in_), R, W)

    def ld(self, buf, out, in_, R, q="sp", W=None):
        return self.dma(q, [lambda e: e.dma_start(out=out, in_=in_)], buf, reads=R, writes=[buf] if W is None else W)

    def st(self, buf, out, in_, W, q="sp", R=None):
        return self.dma(q, [lambda e: e.dma_start(out=out, in_=in_)], buf, reads=[buf] if R is None else R, writes=W)


def _bc(ap, n=128):
    return ap.partition_broadcast(n)


def build(debug=False, stop=99, gather=True):
    nc = bass.Bass("TRN2", target_bir_lowering=False)
    del _ALL_BUFS[:]
    with ExitStack() as st:
        st.enter_context(nc.allow_non_contiguous_dma(reason="small parameter layouts"))
        S = Sched(nc, st)
        _IN = {
            "xs": ("xs", [L, D]),
            "ctxs": ("ctxs", [LC, D]),
            "cvec": ("cvec", [128, 8, 2]),
            "w_mod": ("w_mod", [D, 6 * D]),
            "b_modT": ("b_modT", [128, 48]),
            "n1T": ("n1T", [128, 8]),
            "n2T": ("n2T", [128, 8]),
            "w_in": ("w_in", [D, DIN]),
            "ident_d": ("ident", [128, 128]),
            "qg": ("qg", [1, 64]),
            "kg": ("kg", [1, 64]),
            "ropec": ("ropec", [L, 32]),
            "ropes": ("ropes", [L, 32]),
            "lam_re": ("lam_re", [64, 64]),
            "lam_im": ("lam_im", [64, 64]),
            "lstep": ("lstep", [64, 1]),
            "bT_re": ("bT_re", [8, 128, 64]),
            "bT_im": ("bT_im", [8, 128, 64]),
            "cst_a": ("cst_a", [8, 128, 128]),
            "cst_b": ("cst_b", [8, 128, 128]),
            "rep_d": ("rep", [8, 64, 128]),
            "rmask_d": ("rmask", [128, 8]),
            "dskipT": ("dskipT", [128, 4]),
            "w_glu": ("w_glu", [512, 512]),
            "b_gluT": ("b_gluT", [128, 4]),
            "w_ssm": ("w_ssm", [512, D]),
            "w_att": ("w_att", [512, D]),
            "w_out": ("w_out", [D, D]),
            "rw_d": ("rw", [D, 32]),
            "rb_d": ("rb", [1, 32]),
            "wgu_d": ("wgu", [32, D, 2048]),
            "wd_d": ("wd", [32, D, D]),
            "wgu_sh": ("wgu_sh", [4 * D, 2048]),
            "wd_sh": ("wd_sh", [4 * D, D]),
            "bguT": ("bguT", [32, 128, 16]),
            "bdn": ("bdn", [32, D]),
            "fg_d": ("fg", [1, D]),
        }

        class _Lazy:
            def __init__(self):
                self.c = {}

            def __getattr__(self, k):
                if k not in self.c:
                    n_, sh_ = _IN[k]
                    self.c[k] = S.dram(n_, sh_, F32, kind="ExternalInput")
                return self.c[k]
        X = _Lazy()
        out_d = S.dram("out", [NOWN, D], F32, kind="ExternalOutput")
        dbg = S.dram("dbg", [128, 1024], F32, kind="ExternalOutput") if debug else None
        uT_d = S.dram("uT_d", [512, NALL], BF16)
        gs_d = S.dram("gs_d", [2048, NOWN], BF16)
        modrow_d = S.dram("modrow_d", [16, 128], F32)

        S.ph = st
        ident = S.sbuf("ident", [128, 128], F32)
        identb = S.sbuf("identb", [128, 128], BF16)
        modT = S.sbuf("modT", [128, 48, 2], F32)
        G1 = S.sbuf("G1", [128, 8, 2], F32)
        G2 = S.sbuf("G2", [128, 8], F32)
        S.ph = None
        kt_d = S.dram("kt_d", [2, 128, NALL], BF16)
        va_d = S.dram("va_d", [128, 66 * 132], BF16)
        qt_d = S.dram("qt_d", [128, 4 * NOWN], BF16)
        yT_d = S.dram("yT_d", [512, NOWN], F32)
        oT_d = S.dram("oT_d", [512, NOWN], BF16)
        xmix_d = S.dram("xmix_d", [NOWN, D], F32)
        h2T_d = S.dram("h2T_d", [D, NOWN], BF16)
        gT_d = S.dram("gT_d", [32, NOWN], F32)

        if gather:
            wgu_b = S.dram("wgu_b", [4 * D, 2048], F32); wd_b = S.dram("wd_b", [4 * D, D], F32)
            wgu_all = S.dram("wgu_all", [32 * D, 2048], F32); wd_all = S.dram("wd_all", [32 * D, D], F32)
            WGU = lambda e, r0, r1, c0, c1: wgu_all.h[e * D + r0:e * D + r1, c0:c1]
            WD = lambda e, r0, r1, c0, c1: wd_all.h[e * D + r0:e * D + r1, c0:c1]
            wgu_src, wd_src = wgu_all, wd_all
        else:
            WGU = lambda e, r0, r1, c0, c1: X.wgu_d.h[e, r0:r1, c0:c1]
            WD = lambda e, r0, r1, c0, c1: X.wd_d.h[e, r0:r1, c0:c1]

        with S.phase():
            if gather:
                for e4 in range(4):
                    S.dma("pool", [lambda en, e4=e4: en.dma_start(out=wgu_b.h[e4 * D:(e4 + 1) * D, :], in_=X.wgu_sh.h[e4 * D:(e4 + 1) * D, :])], wgu_b, reads=[X.wgu_sh], writes=[wgu_b])
                    S.dma("pool", [lambda en, e4=e4: en.dma_start(out=wd_b.h[e4 * D:(e4 + 1) * D, :], in_=X.wd_sh.h[e4 * D:(e4 + 1) * D, :])], wd_b, reads=[X.wd_sh], writes=[wd_b])
                rg = [list(range(8))]
                S.dma("pool", [lambda en: en.collective_compute("AllGather", ALU.bypass, replica_groups=rg, ins=[wgu_b.h.opt()], outs=[wgu_all.h.opt()])], wgu_all, reads=[wgu_b], writes=[wgu_all])
                S.dma("pool", [lambda en: en.collective_compute("AllGather", ALU.bypass, replica_groups=rg, ins=[wd_b.h.opt()], outs=[wd_all.h.opt()])], wd_all, reads=[wd_b], writes=[wd_all])
            cv = S.sbuf("cv", [128, 8, 2], F32)
            sg = S.sbuf("sg", [128, 8, 2], F32)
            bm = S.sbuf("bm", [128, 48], F32)
            n1 = S.sbuf("n1", [128, 8], F32)
            n2 = S.sbuf("n2", [128, 8], F32)
            S.ld(ident, ident[:], X.ident_d.h[:, :], [X.ident_d])
            S.cp("dve", identb[:], ident[:], [ident], [identb])
            S.ld(cv, cv[:], X.cvec.h[:, :, :], [X.cvec])
            S.ld(bm, bm[:], X.b_modT.h[:, :], [X.b_modT])
            S.ld(n1, n1[:], X.n1T.h[:, :], [X.n1T])
            S.ld(n2, n2[:], X.n2T.h[:, :], [X.n2T])
            S.act(sg[:], cv[:], AF.Sigmoid, [cv], [sg])
            S.tt("dve", cv[:], cv[:], sg[:], ALU.mult, [cv, sg], [cv])
            wm = [S.sbuf("wm", [128, 8, 512], F32) for _ in range(2)]
            pm = S.psum("pm", [128, 48, 2], F32)
            for blk in range(12):
                w = wm[blk % 2]
                S.ld(w, w[:], X.w_mod.h[:, blk * 512:(blk + 1) * 512].rearrange("(k p) c -> p k c", p=128), [X.w_mod])
                for mi in range(4):
                    m = blk * 4 + mi
                    for kc in range(8):
                        S.mm(pm[:, m, :], w[:, kc, mi * 128:(mi + 1) * 128], cv[:, kc, :], kc == 0, kc == 7, [w, cv], [pm])
            for j in range(2):
                S.tt("dve", modT[:, :, j], pm[:, :, j], bm[:], ALU.add, [pm, bm], [modT])
            for j in range(2):
                S.stt(G1[:, :, j], modT[:, 8:16, j], 1.0, n1[:], ALU.add, ALU.mult, [modT, n1], [G1])
            S.stt(G2[:], modT[:, 32:40, 0], 1.0, n2[:], ALU.add, ALU.mult, [modT, n2], [G2])
            gcol = S.sbuf("gcol", [128, 16], F32)
            S.cp("dve", gcol[:, 0:8], modT[:, 16:24, 0], [modT], [gcol])
            S.cp("dve", gcol[:, 8:16], modT[:, 40:48, 0], [modT], [gcol])
            pg = S.psum("pg", [16, 128], F32)
            S.tr(pg[:], gcol[:], ident[:], [gcol, ident], [pg])
            grow = S.sbuf("grow", [16, 128], F32)
            S.cp("dve", grow[:], pg[:], [pg], [grow])
            S.st(grow, modrow_d.h[:, :], grow[:], [modrow_d])
            if debug == 10:
                dt_ = S.sbuf("dt_", [128, 1024], F32)
                S.op("pool", lambda e: e.memset(dt_[:], 0.0), (), [dt_])
                S.cp("dve", dt_[:, 0:96], modT[:].rearrange("p a b -> p (a b)"), [modT], [dt_])
                S.st(dt_, dbg.h[:, :], dt_[:], [dbg])
            S.end_phase()
        if debug == 10 or stop == 0:
            return nc

        with S.phase():
            ktt = [S.sbuf("ktt", [128, 2, 128], BF16) for _ in range(2)]
            vat = [S.sbuf("vat", [128, 2, 66], BF16) for _ in range(2)]
            qtt = [S.sbuf("qtt", [128, 4, 128], BF16) for _ in range(2)]
            wst = S.sbuf("wst", [128, 8, 104], F32)
            wbf = S.sbuf("wbf", [128, 8, DIN], BF16)
            for cb in range(32):
                S.ld(wst, wst[:], X.w_in.h[:, cb * 104:(cb + 1) * 104].rearrange("(k p) c -> p k c", p=128), [X.w_in])
                S.cp("pool" if cb % 2 else "act", wbf[:, :, cb * 104:(cb + 1) * 104], wst[:], [wst], [wbf])
            qgt = S.sbuf("qgt", [128, 64], F32)
            kgt = S.sbuf("kgt", [128, 64], F32)
            S.ld(qgt, qgt[:], _bc(X.qg.h[0:1, :]), [X.qg])
            S.ld(kgt, kgt[:], _bc(X.kg.h[0:1, :]), [X.kg])
            S.ts("dve", qgt[:], qgt[:], 0.125, None, ALU.mult, None, [qgt], [qgt])
            for v_ in vat:
                S.op("pool", lambda e, v_=v_: e.memset(v_[:], 1.0), (), [v_])
            xt = [S.sbuf("xt", [128, D], F32) for _ in range(2)]
            sq = S.sbuf("sq", [128, D], BF16)
            ss = [S.sbuf("ss", [128, 1], F32) for _ in range(2)]
            xn = [S.sbuf("xn", [128, D], BF16) for _ in range(2)]
            hT = [S.sbuf("hT", [128, 8, 512], BF16) for _ in range(2)]
            pT = [S.psum("pT", [128, D], BF16) for _ in range(1)]
            pf = [S.psum("pf", [128, 512], F32) for _ in range(2)]
            kvq = S.sbuf("kvq", [128, 6, 512], BF16)
            uo = [S.sbuf("uo", [128, 4, 512], BF16) for _ in range(1)]
            go = [S.sbuf("go", [128, 8, 512], BF16) for _ in range(1)]
            rc = [S.sbuf("rc", [128, 32], F32) for _ in range(2)]
            rs = [S.sbuf("rs", [128, 32], F32) for _ in range(2)]
            qk = [S.sbuf("qk", [128, 10, 64], F32) for _ in range(2)]
            qsq = S.sbuf("qsq", [128, 10, 64], F32)
            qss = [S.sbuf("qss", [128, 10], F32) for _ in range(2)]
            t1 = S.sbuf("t1", [128, 10, 32], F32)
            t2 = S.sbuf("t2", [128, 10, 32], F32)
            qr = [S.sbuf("qr", [128, 12, 64], BF16) for _ in range(2)]
            nsup = NALL // 512 + 1
            cnt = 0
            P1CUT = int(os.environ.get("P1CUT", "9"))
            for su in range(17 if P1CUT > 0 else 0):
                S.maybe_sync()
                ntile = 2 if su == 0 else 4
                tok0 = 0 if su == 0 else LC + (su - 1) * 512
                own = 1 <= su <= 8
                j = 1 if su == 0 else 0
                h = hT[su % 2]
                for sb_ in range(ntile):
                    x_ = xt[cnt % 2]
                    s_ = ss[cnt % 2]
                    n_ = xn[cnt % 2]
                    p_ = pT[0]
                    src = X.ctxs.h[sb_ * 128:(sb_ + 1) * 128, :] if su == 0 else X.xs.h[(su - 1) * 512 + sb_ * 128:(su - 1) * 512 + (sb_ + 1) * 128, :]
                    S.ld(x_, x_[:], src, [X.ctxs if su == 0 else X.xs])
                    S.act(sq[:], x_[:], AF.Square, [x_], [sq, s_], accum=s_[:])
                    S.ts("dve", s_[:], s_[:], 1.0 / D, EPS, ALU.mult, ALU.add, [s_], [s_])
                    S.act(s_[:], s_[:], AF.Sqrt, [s_], [s_])
                    S.op("dve", lambda e, s_=s_: e.reciprocal(out=s_[:], in_=s_[:]), [s_], [s_])
                    S.ts("dve", n_[:], x_[:], s_[:, 0:1], None, ALU.mult, None, [x_, s_], [n_])
                    for kc in range(8):
                        S.tr(p_[:, kc * 128:(kc + 1) * 128], n_[:, kc * 128:(kc + 1) * 128], identb[:], [n_, identb], [p_])
                    for kc in range(8):
                        o_ = h[:, kc, sb_ * 128:(sb_ + 1) * 128]
                        i_ = p_[:, kc * 128:(kc + 1) * 128]
                        if kc % 2 == 0:
                            S.act(o_, i_, AF.Identity, [p_, G1, modT], [h], bias=modT[:, kc, j:j + 1], scale=G1[:, kc, j:j + 1])
                        else:
                            S.ts("dve", o_, i_, G1[:, kc, j:j + 1], modT[:, kc, j:j + 1], ALU.mult, ALU.add, [p_, G1, modT], [h])
                    cnt += 1
                ncol = ntile * 128
                if P1CUT < 2:
                    continue
                u_ = uo[0]
                for mo in range(4):
                    p = pf[mo % 2]
                    for kc in range(8):
                        S.mm(p[:, 0:ncol], wbf[:, kc, mo * 128:(mo + 1) * 128], h[:, kc, 0:ncol], kc == 0, kc == 7, [wbf, h], [p])
                    S.cp("act" if mo % 2 else "dve", u_[:, mo, 0:ncol], p[:, 0:ncol], [p], [u_])
                S.st(u_, uT_d.h[:, tok0:tok0 + ncol].rearrange("(m p) t -> p m t", p=128), u_[:, :, 0:ncol], [uT_d])
                if own:
                    g_ = go[0]
                    o0 = (su - 1) * 512
                    for gh in range(2):
                        for mo in range(8):
                            p = pf[mo % 2]
                            c0 = 1280 + (gh * 8 + mo) * 128
                            for kc in range(8):
                                S.mm(p[:], wbf[:, kc, c0:c0 + 128], h[:, kc, :], kc == 0, kc == 7, [wbf, h], [p])
                            S.act(g_[:, mo, :], p[:], AF.Sigmoid, [p], [g_])
                        S.st(g_, gs_d.h[gh * 1024:(gh + 1) * 1024, o0:o0 + 512].rearrange("(m p) t -> p m t", p=128), g_[:], [gs_d])
                if P1CUT < 3:
                    continue
                ncq = 6 if own else 2
                for c in range(ncq):
                    p = pf[c % 2]
                    c0 = 512 + c * 128
                    for kc in range(8):
                        S.mm(p[:, 0:ncol], wbf[:, kc, c0:c0 + 128], h[:, kc, 0:ncol], kc == 0, kc == 7, [wbf, h], [p])
                    S.cp("act" if c % 2 else "dve", kvq[:, c, 0:ncol], p[:, 0:ncol], [p], [kvq])
                P1SUB = int(os.environ.get("P1SUB", "9"))
                for sb_ in range(ntile if P1SUB >= 2 else 0):
                    ti = (0 if su == 0 else 2 + (su - 1) * 4) + sb_
                    lat = su > 0
                    q_ = qk[ti % 2]
                    pt_ = pT[0]
                    for c in range(ncq):
                        S.tr(pt_[:, c * 128:(c + 1) * 128], kvq[:, c, sb_ * 128:(sb_ + 1) * 128], identb[:], [kvq, identb], [pt_])
                    if P1SUB < 3:
                        continue
                    S.cp("act", q_[:, 8:10, :], pt_[:, 0:128].rearrange("p (a b) -> p a b", b=64), [pt_], [q_])
                    if P1SUB < 4:
                        continue
                    va_ = vat[ti % 2]
                    S.cp("dve", va_[:, :, 0:64], pt_[:, 128:256].rearrange("p (a b) -> p a b", b=64), [pt_], [va_])
                    if P1SUB < 5:
                        continue
                    S.st(va_, va_d.h[:, ti * 132:(ti + 1) * 132], va_[:].rearrange("p a b -> p (a b)"), [va_d])
                    h0 = 8
                    if own:
                        S.cp("act", q_[:, 0:8, :], pt_[:, 256:768].rearrange("p (a b) -> p a b", b=64), [pt_], [q_])
                        h0 = 0
                    if P1CUT < 4:
                        continue
                    nh = 10 - h0
                    s2 = qss[ti % 2]
                    S.tt("dve", qsq[:, h0:10, :], q_[:, h0:10, :], q_[:, h0:10, :], ALU.mult, [q_], [qsq])
                    S.op("dve", lambda e, s2=s2, h0=h0: e.tensor_reduce(out=s2[:, h0:10], in_=qsq[:, h0:10, :], axis=AX.X, op=ALU.add), [qsq], [s2])
                    S.ts("dve", s2[:, h0:10], s2[:, h0:10], 1.0 / 64, EPS, ALU.mult, ALU.add, [s2], [s2])
                    S.act(s2[:, h0:10], s2[:, h0:10], AF.Sqrt, [s2], [s2])
                    S.op("dve", lambda e, s2=s2, h0=h0: e.reciprocal(out=s2[:, h0:10], in_=s2[:, h0:10]), [s2], [s2])
                    S.tt("dve", q_[:, h0:10, :], q_[:, h0:10, :], s2[:, h0:10].unsqueeze(2).to_broadcast([128, nh, 64]), ALU.mult, [q_, s2], [q_])
                    if own:
                        S.tt("dve", q_[:, 0:8, :], q_[:, 0:8, :], qgt[:].unsqueeze(1).to_broadcast([128, 8, 64]), ALU.mult, [q_, qgt], [q_])
                    S.tt("dve", q_[:, 8:10, :], q_[:, 8:10, :], kgt[:].unsqueeze(1).to_broadcast([128, 2, 64]), ALU.mult, [q_, kgt], [q_])
                    if P1CUT < 5:
                        continue
                    r_ = qr[ti % 2]
                    if lat:
                        c_ = rc[ti % 2]
                        sn = rs[ti % 2]
                        r0 = (su - 1) * 512 + sb_ * 128
                        S.ld(c_, c_[:], X.ropec.h[r0:r0 + 128, :], [X.ropec])
                        S.ld(sn, sn[:], X.ropes.h[r0:r0 + 128, :], [X.ropes])
                        ev_ = q_[:, h0:10, 0::2]
                        od_ = q_[:, h0:10, 1::2]
                        cb_ = c_[:].unsqueeze(1).to_broadcast([128, nh, 32])
                        sb2 = sn[:].unsqueeze(1).to_broadcast([128, nh, 32])
                        S.tt("dve", t1[:, h0:10, :], ev_, cb_, ALU.mult, [q_, c_], [t1])
                        S.tt("dve", t2[:, h0:10, :], od_, sb2, ALU.mult, [q_, sn], [t2])
                        S.tt("dve", t1[:, h0:10, :], t1[:, h0:10, :], t2[:, h0:10, :], ALU.subtract, [t1, t2], [t1])
                        S.tt("dve", t2[:, h0:10, :], ev_, sb2, ALU.mult, [q_, sn], [t2])
                        S.tt("dve", od_, od_, cb_, ALU.mult, [q_, c_], [q_])
                        S.tt("dve", od_, od_, t2[:, h0:10, :], ALU.add, [q_, t2], [q_])
                        S.cp("dve", ev_, t1[:, h0:10, :], [t1], [q_])
                    if own:
                        S.cp("act", r_[:, 0:8, :], q_[:, 0:8, :], [q_], [r_])
                    S.cp("dve", r_[:, 8:12:2, :], q_[:, 8:10, :], [q_], [r_])
                    S.cp("dve", r_[:, 9:12:2, :], q_[:, 8:10, :], [q_], [r_])
                    if P1CUT < 6:
                        continue
                    pt_ = pT[0]
                    kt_ = ktt[ti % 2]
                    for kv in range(2):
                        S.tr(pt_[:, kv * 128:(kv + 1) * 128], r_[:, 8 + 2 * kv:10 + 2 * kv, :].rearrange("p a b -> p (a b)"), identb[:], [r_, identb], [pt_])
                    S.cp("dve", kt_[:], pt_[:, 0:256].rearrange("p (a b) -> p a b", b=128), [pt_], [kt_])
                    S.st(kt_, kt_d.h[:, :, ti * 128:(ti + 1) * 128].rearrange("k p t -> p k t"), kt_[:], [kt_d])
                    if own:
                        o0 = (su - 1) * 512 + sb_ * 128
                        qt_ = qtt[ti % 2]
                        for pc in range(4):
                            S.tr(pt_[:, 256 + pc * 128:256 + (pc + 1) * 128], r_[:, 2 * pc:2 * pc + 2, :].rearrange("p a b -> p (a b)"), identb[:], [r_, identb], [pt_])
                        S.cp("act", qt_[:], pt_[:, 256:768].rearrange("p (a b) -> p a b", b=128), [pt_], [qt_])
                        S.st(qt_, qt_d.h.rearrange("p (a t) -> p a t", a=4)[:, :, o0:o0 + 128], qt_[:], [qt_d])
            if debug == 1:
                dt_ = S.sbuf("dt_", [128, 1024], F32)
                S.op("pool", lambda e: e.memset(dt_[:], 0.0), (), [dt_])
                S.cp("dve", dt_[:, 0:96], modT[:].rearrange("p a b -> p (a b)"), [modT], [dt_])
                kb_ = S.sbuf("kb_", [128, 128], BF16); qb_ = S.sbuf("qb_", [128, 128], BF16); vb_ = S.sbuf("vb_", [128, 132], BF16); k1_ = S.sbuf("k1_", [128, 128], BF16)
                S.ld(kb_, kb_[:], kt_d.h[0, :, 256:384], [kt_d]); S.ld(qb_, qb_[:], qt_d.h[:, 0:128], [qt_d])
                S.ld(vb_, vb_[:], va_d.h[:, 2 * 132:3 * 132], [va_d]); S.ld(k1_, k1_[:], kt_d.h[1, :, 0:128], [kt_d])
                S.op("dve", lambda e: e.tensor_copy(out=dt_[:, 128:256], in_=kb_[:]), [kb_], [dt_])
                S.op("dve", lambda e: e.tensor_copy(out=dt_[:, 256:384], in_=qb_[:]), [qb_], [dt_])
                S.op("dve", lambda e: e.tensor_copy(out=dt_[:, 384:516], in_=vb_[:]), [vb_], [dt_])
                S.op("dve", lambda e: e.tensor_copy(out=dt_[:, 640:768], in_=k1_[:]), [k1_], [dt_])
                S.st(dt_, dbg.h[:, :], dt_[:], [dbg])
            S.end_phase()
        if stop == 1:
            return nc

        with S.phase():
            NG = 32 if not (2 <= debug < 10) else 2
            lr = S.sbuf("lr", [64, 64], F32); li = S.sbuf("li", [64, 64], F32); lsp = S.sbuf("lsp", [64, 1], F32)
            S.ld(lr, lr[:], X.lam_re.h[:, :], [X.lam_re]); S.ld(li, li[:], X.lam_im.h[:, :], [X.lam_im]); S.ld(lsp, lsp[:], X.lstep.h[:, :], [X.lstep])
            rmask = S.sbuf("rmask", [128, 8], F32); S.ld(rmask, rmask[:], X.rmask_d.h[:, :], [X.rmask_d])
            dsk = S.sbuf("dsk", [128, 4], F32); S.ld(dsk, dsk[:], X.dskipT.h[:, :], [X.dskipT])
            sgn = S.sbuf("sgn", [128, 1], F32)
            S.op("dve", lambda e: e.memset(sgn[0:64, :], 1.0), (), [sgn]); S.op("dve", lambda e: e.memset(sgn[64:128, :], -1.0), (), [sgn])
            hpi = S.sbuf("hpi", [64, 1], F32); S.op("pool", lambda e: e.memset(hpi[:], float(np.pi / 2)), (), [hpi])
            zer = S.sbuf("zer", [64, 1], F32); S.op("pool", lambda e: e.memset(zer[:], 0.0), (), [zer])
            S.act(lsp[:], lsp[:], AF.Exp, [lsp], [lsp])
            ar = S.sbuf("ar", [64, 64], F32); th = S.sbuf("th", [64, 64], F32)
            S.ts("dve", ar[:], lr[:], lsp[:, 0:1], None, ALU.mult, None, [lr, lsp], [ar])
            S.ts("dve", th[:], li[:], lsp[:, 0:1], None, ALU.mult, None, [li, lsp], [th])
            rr = S.sbuf("rr", [64, 128], F32)
            S.act(rr[:, 0:64], ar[:], AF.Exp, [ar], [rr])
            CK = S.sbuf("CK", [64, 14, 128], F32); SK = S.sbuf("SK", [64, 14, 128], F32)
            cc = S.sbuf("cc", [64, 64], F32); s2_ = S.sbuf("s2_", [64, 64], F32)
            c_ = S.sbuf("c_", [64, 64], F32); s_ = S.sbuf("s_", [64, 64], F32)
            S.act(c_[:], th[:], AF.Sin, [th, hpi], [c_], bias=hpi[:, 0:1], scale=1.0 / 32)
            S.act(s_[:], th[:], AF.Sin, [th, zer], [s_], bias=zer[:, 0:1], scale=1.0 / 32)
            def dbl(co, so, ci, si):
                S.tt("dve", cc[:], ci, ci, ALU.mult, [c_, CK], [cc])
                S.tt("dve", s2_[:], si, si, ALU.mult, [s_, SK], [s2_])
                S.stt(so, ci, 2.0, si, ALU.mult, ALU.mult, [c_, s_, CK, SK], [s_, SK])
                S.tt("dve", co, cc[:], s2_[:], ALU.subtract, [cc, s2_], [c_, CK])
            for _ in range(4):
                dbl(c_[:], s_[:], c_[:], s_[:])
            dbl(CK[:, 0, 0:64], SK[:, 0, 0:64], c_[:], s_[:])
            for k in range(1, 14):
                dbl(CK[:, k, 0:64], SK[:, k, 0:64], CK[:, k - 1, 0:64], SK[:, k - 1, 0:64])
            S.cp("pool", CK[:, :, 64:128], CK[:, :, 0:64], [CK], [CK]); S.cp("pool", SK[:, :, 64:128], SK[:, :, 0:64], [SK], [SK])
            S.cp("pool", rr[:, 64:128], rr[:, 0:64], [rr], [rr])
            Lr = S.sbuf("Lr", [64, 64], F32); Li = S.sbuf("Li", [64, 64], F32)
            S.tt("dve", Lr[:], rr[:, 0:64], CK[:, 0, 0:64], ALU.mult, [rr, CK], [Lr])
            S.tt("dve", Li[:], rr[:, 0:64], SK[:, 0, 0:64], ALU.mult, [rr, SK], [Li])
            S.ts("dve", Lr[:], Lr[:], -1.0, None, ALU.add, None, [Lr], [Lr])
            den = S.sbuf("den", [64, 64], F32); tq = S.sbuf("tq", [64, 64], F32)
            S.tt("dve", den[:], lr[:], lr[:], ALU.mult, [lr], [den]); S.tt("dve", tq[:], li[:], li[:], ALU.mult, [li], [tq])
            S.tt("dve", den[:], den[:], tq[:], ALU.add, [den, tq], [den])
            S.op("dve", lambda e: e.reciprocal(out=den[:], in_=den[:]), [den], [den])
            cfr = S.sbuf("cfr", [64, 64], F32); cfi = S.sbuf("cfi", [64, 64], F32)
            S.tt("dve", cfr[:], Lr[:], lr[:], ALU.mult, [Lr, lr], [cfr]); S.tt("dve", tq[:], Li[:], li[:], ALU.mult, [Li, li], [tq])
            S.tt("dve", cfr[:], cfr[:], tq[:], ALU.add, [cfr, tq], [cfr]); S.tt("dve", cfr[:], cfr[:], den[:], ALU.mult, [cfr, den], [cfr])
            S.tt("dve", cfi[:], Li[:], lr[:], ALU.mult, [Li, lr], [cfi]); S.tt("dve", tq[:], Lr[:], li[:], ALU.mult, [Lr, li], [tq])
            S.tt("dve", cfi[:], cfi[:], tq[:], ALU.subtract, [cfi, tq], [cfi]); S.tt("dve", cfi[:], cfi[:], den[:], ALU.mult, [cfi, den], [cfi])
            PCs = S.sbuf("PCs", [128, 14, 64], F32); PSs = S.sbuf("PSs", [128, 14, 64], F32); Rs = S.sbuf("Rs", [128, 64], F32)
            ptr = [S.psum("ptr", [128, 512], F32) for _ in range(2)]
            for k in range(14):
                for w, (src, dst) in enumerate(((CK, PCs), (SK, PSs))):
                    p = ptr[(2 * k + w) % 2]
                    S.tr(p[:, 0:64], src[:, k, :], ident[0:64, 0:64], [src, ident], [p])
                    S.cp("act" if w else "dve", dst[:, k, :], p[:, 0:64], [p], [dst])
            S.tr(ptr[0][:, 0:64], rr[:], ident[0:64, 0:64], [rr, ident], [ptr[0]])
            S.cp("dve", Rs[:], ptr[0][:, 0:64], [ptr[0]], [Rs])
            Bst = S.sbuf("Bst", [128, 16, 128], BF16); Bsw = S.sbuf("Bsw", [128, 16, 128], BF16)
            C1 = S.sbuf("C1", [128, 16, 128], BF16); C2 = S.sbuf("C2", [128, 16, 128], BF16)
            S.op("pool", lambda e: e.memset(C1[:], 0.0), (), [C1]); S.op("pool", lambda e: e.memset(C2[:], 0.0), (), [C2])
            rp = S.sbuf("rp", [64, 128], F32); btr = S.sbuf("btr", [128, 64], F32); bti = S.sbuf("bti", [128, 64], F32)
            crr = S.sbuf("crr", [128, 64], F32); cri = S.sbuf("cri", [128, 64], F32)
            F1 = S.sbuf("F1", [128, 128], F32); F2 = S.sbuf("F2", [128, 128], F32); tb = S.sbuf("tb", [128, 64], F32)
            ca = S.sbuf("ca", [128, 128], F32); cb2 = S.sbuf("cb2", [128, 128], F32)

            def build_mats(ct):
                for d_ in range(2):
                    dct = d_ * 4 + ct
                    S.ld(rp, rp[:], X.rep_d.h[dct, :, :], [X.rep_d]); S.ld(btr, btr[:], X.bT_re.h[dct, :, :], [X.bT_re]); S.ld(bti, bti[:], X.bT_im.h[dct, :, :], [X.bT_im])
                    S.ld(ca, ca[:], X.cst_a.h[dct, :, :], [X.cst_a]); S.ld(cb2, cb2[:], X.cst_b.h[dct, :, :], [X.cst_b])
                    p = ptr[dct % 2]
                    S.mm(p[:, 0:64], rp[:], cfr[:], True, True, [rp, cfr], [p]); S.mm(p[:, 64:128], rp[:], cfi[:], True, True, [rp, cfi], [p])
                    S.cp("act", crr[:], p[:, 0:64], [p], [crr]); S.cp("act", cri[:], p[:, 64:128], [p], [cri])
                    S.tt("dve", F1[:, 0:64], crr[:], btr[:], ALU.mult, [crr, btr], [F1]); S.tt("dve", tb[:], cri[:], bti[:], ALU.mult, [cri, bti], [tb])
                    S.tt("dve", F1[:, 0:64], F1[:, 0:64], tb[:], ALU.subtract, [F1, tb], [F1])
                    S.tt("dve", F1[:, 64:128], crr[:], bti[:], ALU.mult, [crr, bti], [F1]); S.tt("dve", tb[:], cri[:], btr[:], ALU.mult, [cri, btr], [tb])
                    S.tt("dve", F1[:, 64:128], F1[:, 64:128], tb[:], ALU.add, [F1, tb], [F1])
                    S.cp("pool", F2[:, 0:64], F1[:, 64:128], [F1], [F2]); S.ts("pool", F2[:, 64:128], F1[:, 0:64], -1.0, None, ALU.mult, None, [F1], [F2])
                    for gl in range(8):
                        li_ = d_ * 8 + gl
                        S.ts("dve", Bst[:, li_, :], F1[:], rmask[:, gl:gl + 1], None, ALU.mult, None, [F1, rmask], [Bst])
                        S.ts("pool", Bsw[:, li_, :], F2[:], rmask[:, gl:gl + 1], None, ALU.mult, None, [F2, rmask], [Bsw])
                        S.ts("dve", C1[:, li_, gl * 16:(gl + 1) * 16], ca[:, gl * 16:(gl + 1) * 16], sgn[:, 0:1], None, ALU.mult, None, [ca, sgn], [C1])
                        S.ts("pool", C2[:, li_, gl * 16:(gl + 1) * 16], cb2[:, gl * 16:(gl + 1) * 16], -1.0, None, ALU.mult, None, [cb2], [C2])
            Tc = S.sbuf("Tc", [128, NALL], F32); Ts = S.sbuf("Ts", [128, NALL], F32)
            tmpa = S.sbuf("tmpa", [128, 512], F32)
            uTt = S.sbuf("uTt", [128, NALL], BF16)
            Yacc = S.sbuf("Yacc", [128, NOWN], F32)
            Rt = S.sbuf("Rt", [128, 512], F32); ones = S.sbuf("ones", [128, 512], F32)
            S.op("pool", lambda e: e.memset(ones[:], 1.0), (), [ones])
            Ssb = [S.sbuf("Ssb", [128, 512], F32) for _ in range(2)]; Wsb = [S.sbuf("Wsb", [128, 512], F32) for _ in range(2)]
            Ht = [S.sbuf("Ht", [128, 512], F32) for _ in range(2)]
            Hc = [S.sbuf("Hc", [128, 512], BF16) for _ in range(1)]; Hs = [S.sbuf("Hs", [128, 512], BF16) for _ in range(1)]
            pS = [S.psum("pS", [128, 512], F32) for _ in range(2)]; pW = [S.psum("pW", [128, 512], F32) for _ in range(2)]
            pY = [S.psum("pY", [128, 512], F32) for _ in range(2)]
            it = 0
            for ct in range(4):
                if ct * 8 >= NG:
                    S.op("pool", lambda e: e.memset(Yacc[:], 0.0), (), [Yacc])
                    S.st(Yacc, yT_d.h[ct * 128:(ct + 1) * 128, :], Yacc[:], [yT_d])
                    continue
                build_mats(ct)
                S.ld(uTt, uTt[:], uT_d.h[ct * 128:(ct + 1) * 128, :], [uT_d])
                S.ts("dve", Yacc[:], uTt[:, LC:LC + NOWN], dsk[:, ct:ct + 1], None, ALU.mult, None, [uTt, dsk], [Yacc])
                for gl in range(8):
                    for d_ in range(2):
                        S.maybe_sync()
                        gi = d_ * 32 + ct * 8 + gl
                        li_ = d_ * 8 + gl
                        TL = LC + NOWN if d_ == 0 else NALL
                        S.op("pool", lambda e: e.memset(Tc[:, 0:1], 1.0), (), [Tc]); S.op("pool", lambda e: e.memset(Ts[:, 0:1], 0.0), (), [Ts])
                        m = 1; k = 0
                        while m < TL:
                            nn = min(m, TL - m)
                            ck = PCs[:, k, gi:gi + 1]; sk = PSs[:, k, gi:gi + 1]
                            for o in range(0, nn, 512):
                                n = min(512, nn - o)
                                S.ts("pool", tmpa[:, 0:n], Ts[:, o:o + n], sk, None, ALU.mult, None, [Ts, PSs], [tmpa])
                                S.stt(Tc[:, m + o:m + o + n], Tc[:, o:o + n], ck, tmpa[:, 0:n], ALU.mult, ALU.subtract, [Tc, PCs, tmpa], [Tc])
                                S.ts("pool", tmpa[:, 0:n], Tc[:, o:o + n], sk, None, ALU.mult, None, [Tc, PSs], [tmpa])
                                S.stt(Ts[:, m + o:m + o + n], Ts[:, o:o + n], ck, tmpa[:, 0:n], ALU.mult, ALU.add, [Ts, PCs, tmpa], [Ts])
                            m *= 2; k += 1
                        S.ts("pool", Rt[:], ones[:], Rs[:, gi:gi + 1], None, ALU.mult, None, [ones, Rs], [Rt])
                        if d_ == 0:
                            chunks = [(0, LC, 0, False)] + [(LC + 512 * i, 512, LC + 512 * i, False) for i in range(8)]
                        else:
                            chunks = [(0, LC, LC - 1, True)] + [(LC + 512 * i, 512, NALL + LC - 1 - (LC + 512 * i), True) for i in range(15, -1, -1)]
                        prev = None
                        for (lo, n, tau, rev) in chunks:
                            ps_, pw_ = pS[it % 2], pW[it % 2]
                            ss_, ws_, h_ = Ssb[it % 2], Wsb[it % 2], Ht[it % 2]
                            x_ = ss_
                            S.mm(ps_[:, 0:n], Bst[:, li_, :], uTt[:, lo:lo + n], True, True, [Bst, uTt], [ps_])
                            S.mm(pw_[:, 0:n], Bsw[:, li_, :], uTt[:, lo:lo + n], True, True, [Bsw, uTt], [pw_])
                            S.cp("act", ss_[:, 0:n], ps_[:, 0:n], [ps_], [ss_]); S.cp("act", ws_[:, 0:n], pw_[:, 0:n], [pw_], [ws_])
                            if rev:
                                tcs = Tc[:, tau - n + 1:tau + 1][:, ::-1]; tss = Ts[:, tau - n + 1:tau + 1][:, ::-1]
                            else:
                                tcs = Tc[:, tau:tau + n]; tss = Ts[:, tau:tau + n]
                            me = "dve" if rev else "pool"
                            S.tt(me, ss_[:, 0:n], ss_[:, 0:n], tcs, ALU.mult, [ss_, Tc], [ss_])
                            S.tt(me, ws_[:, 0:n], ws_[:, 0:n], tss, ALU.mult, [ws_, Ts], [ws_])
                            S.tt("pool" if rev else "dve", x_[:, 0:n], ss_[:, 0:n], ws_[:, 0:n], ALU.add, [ss_, ws_], [x_])
                            if prev is None:
                                init = 0.0; rd = [Rt, x_]
                            else:
                                ph_, pn = prev
                                init = ph_[:, 0:1] if rev else ph_[:, pn - 1:pn]; rd = [Rt, x_, ph_]
                            if rev:
                                S.op("dve", lambda e, o=h_[:, 0:n][:, ::-1], a=Rt[:, 0:n], b=x_[:, 0:n][:, ::-1], i=init: e.tensor_tensor_scan(out=o, data0=a, data1=b, initial=i, op0=ALU.mult, op1=ALU.add), rd, [h_])
                            else:
                                S.op("dve", lambda e, o=h_[:, 0:n], a=Rt[:, 0:n], b=x_[:, 0:n], i=init: e.tensor_tensor_scan(out=o, data0=a, data1=b, initial=i, op0=ALU.mult, op1=ALU.add), rd, [h_])
                            prev = (h_, n)
                            if LC <= lo < LC + NOWN:
                                hc_, hs_, py_ = Hc[0], Hs[0], pY[it % 2]
                                S.tt(me, hc_[:, 0:n], h_[:, 0:n], tcs, ALU.mult, [h_, Tc], [hc_])
                                S.tt(me, hs_[:, 0:n], h_[:, 0:n], tss, ALU.mult, [h_, Ts], [hs_])
                                S.mm(py_[:, 0:n], C1[:, li_, :], hc_[:, 0:n], True, False, [C1, hc_], [py_])
                                S.mm(py_[:, 0:n], C2[:, li_, :], hs_[:, 0:n], False, True, [C2, hs_], [py_])
                                S.tt("dve", Yacc[:, lo - LC:lo - LC + n], Yacc[:, lo - LC:lo - LC + n], py_[:, 0:n], ALU.add, [Yacc, py_], [Yacc])
                            it += 1
                S.st(Yacc, yT_d.h[ct * 128:(ct + 1) * 128, :], Yacc[:], [yT_d])
            S.end_phase()
        with S.phase():
            NQB = 8 if not (2 <= debug < 10) else 1
            KT2 = [S.sbuf("KT2", [128, NALL], BF16) for _ in range(2)]
            VA = S.sbuf("VA", [128, 66, 2, 66], BF16)
            QT = S.sbuf("QT", [128, 4, NOWN], BF16)
            for kv in range(2):
                S.ld(KT2[kv], KT2[kv][:], kt_d.h[kv, :, :], [kt_d])
            S.ld(VA, VA[:].rearrange("p a b c -> p (a b c)"), va_d.h[:, :], [va_d])
            S.ld(QT, QT[:].rearrange("p a b -> p (a b)"), qt_d.h[:, :], [qt_d])
            pS = [S.psum("pS", [128, 512], F32) for _ in range(3)]
            pO = [S.psum("pO", [128, 4, 128], F32) for _ in range(2)]
            pR = S.psum("pR", [128, 1024], BF16)
            PT = [S.sbuf("PT", [128, 512], BF16) for _ in range(3)]
            otok = [S.sbuf("otok", [128, 4, 512], BF16) for _ in range(2)]
            oTs = [S.sbuf("oTs", [128, 4, 512], BF16) for _ in range(2)]
            rec = [S.sbuf("rec", [128, 4], F32) for _ in range(2)]
            it = 0
            for qb in range(NQB):
                ot = otok[qb % 2]
                for h in range(8):
                    S.maybe_sync()
                    kv, pc, base = h // 4, h // 2, 64 * (h % 2)
                    po = pO[h % 2]
                    for kt in range(66):
                        ps_, pt_ = pS[it % 3], PT[it % 3]
                        S.mm(ps_[:], KT2[kv][base:base + 64, kt * 128:(kt + 1) * 128], QT[base:base + 64, pc, qb * 512:(qb + 1) * 512], True, True, [KT2[kv], QT], [ps_])
                        S.act(pt_[:], ps_[:], AF.Exp, [ps_], [pt_])
                        for sub in range(4):
                            S.mm(po[:, sub, 0:65], pt_[:, sub * 128:(sub + 1) * 128], VA[:, kt, kv, 0:65], kt == 0 and sub == 0, kt == 65 and sub == 3, [pt_, VA], [po])
                        it += 1
                    rc_ = rec[h % 2]
                    S.op("dve", lambda e, rc_=rc_, po=po: e.reciprocal(out=rc_[:], in_=po[:, :, 64]), [po], [rc_])
                    S.tt("dve", ot[:, :, h * 64:(h + 1) * 64], po[:, :, 0:64], rc_[:].unsqueeze(2).to_broadcast([128, 4, 64]), ALU.mult, [po, rc_], [ot])
                o2 = oTs[qb % 2]
                for sub in range(4):
                    for c in range(4):
                        S.tr(pR[:, c * 128:(c + 1) * 128], ot[:, sub, c * 128:(c + 1) * 128], identb[:], [ot, identb], [pR])
                    S.cp("act" if sub % 2 else "dve", o2[:, :, sub * 128:(sub + 1) * 128], pR[:, 0:512].rearrange("p (a b) -> p a b", b=128), [pR], [o2])
                S.st(o2, oT_d.h[:, qb * 512:(qb + 1) * 512].rearrange("(m p) t -> p m t", p=128), o2[:], [oT_d])
            S.end_phase()
        with S.phase():
            NST = 8 if not (2 <= debug < 10) else 1
            stg = S.sbuf("stg", [128, 8, 512], F32)
            wgl = S.sbuf("wgl", [128, 4, 512], BF16); wss = S.sbuf("wss", [128, 4, D], BF16)
            wat = S.sbuf("wat", [128, 4, D], BF16); wou = S.sbuf("wou", [128, 8, D], BF16)
            S.ld(stg, stg[:, 0:4, :], X.w_glu.h.rearrange("(k p) c -> p k c", p=128), [X.w_glu]); S.cp("act", wgl[:], stg[:, 0:4, :], [stg], [wgl])
            for hf in range(2):
                S.ld(stg, stg[:, 0:4, :], X.w_ssm.h[:, hf * 512:(hf + 1) * 512].rearrange("(k p) c -> p k c", p=128), [X.w_ssm]); S.cp("pool", wss[:, :, hf * 512:(hf + 1) * 512], stg[:, 0:4, :], [stg], [wss])
                S.ld(stg, stg[:, 0:4, :], X.w_att.h[:, hf * 512:(hf + 1) * 512].rearrange("(k p) c -> p k c", p=128), [X.w_att]); S.cp("act", wat[:, :, hf * 512:(hf + 1) * 512], stg[:, 0:4, :], [stg], [wat])
                S.ld(stg, stg[:], X.w_out.h[:, hf * 512:(hf + 1) * 512].rearrange("(k p) c -> p k c", p=128), [X.w_out]); S.cp("pool", wou[:, :, hf * 512:(hf + 1) * 512], stg[:], [stg], [wou])
            rwt = S.sbuf("rwt", [128, 8, 32], F32); S.ld(rwt, rwt[:], X.rw_d.h.rearrange("(k p) c -> p k c", p=128), [X.rw_d])
            rbt = S.sbuf("rbt", [128, 32], F32); S.ld(rbt, rbt[:], _bc(X.rb_d.h[0:1, :]), [X.rb_d])
            bgl = S.sbuf("bgl", [128, 4], F32); S.ld(bgl, bgl[:], X.b_gluT.h[:, :], [X.b_gluT])
            g1r = S.sbuf("g1r", [128, D], F32)
            S.ld(g1r, g1r[:], _bc(modrow_d.h[0:8, :].rearrange("a b -> (a b)").unsqueeze(0)), [modrow_d])
            yt = S.sbuf("yt", [128, 4, 512], F32); ot_ = S.sbuf("ot_", [128, 4, 512], BF16); gst = S.sbuf("gst", [128, 16, 512], BF16)
            x2 = S.sbuf("x2", [128, 4, 512], F32); sT = S.sbuf("sT", [128, 4, 512], BF16); s2T = S.sbuf("s2T", [128, 4, 512], BF16)
            sig = S.sbuf("sig", [128, 512], F32); mT = S.sbuf("mT", [128, 8, 512], BF16)
            ta = S.sbuf("ta", [128, 512], F32); tb2 = S.sbuf("tb2", [128, 512], F32)
            xt = [S.sbuf("xt", [128, D], F32) for _ in range(2)]; xm = [S.sbuf("xm", [128, D], F32) for _ in range(2)]
            sq = S.sbuf("sq", [128, D], BF16); ss = [S.sbuf("ss", [128, 1], F32) for _ in range(2)]
            xn = S.sbuf("xn", [128, D], F32); h2f = S.sbuf("h2f", [128, 8, 128], F32); h2b = [S.sbuf("h2b", [128, 8, 128], BF16) for _ in range(2)]
            lg = S.sbuf("lg", [128, 32], F32); t8 = S.sbuf("t8", [128, 8], F32); msk = S.sbuf("msk", [128, 32], F32)
            ex = S.sbuf("ex", [128, 32], F32); sm = S.sbuf("sm", [128, 1], F32); gsb = S.sbuf("gsb", [32, 128], F32)
            pA = [S.psum("pA", [128, 512], F32) for _ in range(2)]; pB = [S.psum("pB", [128, 512], F32) for _ in range(2)]
            pX = [S.psum("pX", [128, 512], F32) for _ in range(2)]; pL = S.psum("pL", [128, 512], F32)
            for st_ in range(NST):
                S.maybe_sync()
                c0 = st_ * 512
                S.ld(yt, yt[:], yT_d.h[:, c0:c0 + 512].rearrange("(m p) t -> p m t", p=128), [yT_d])
                S.ld(ot_, ot_[:], oT_d.h[:, c0:c0 + 512].rearrange("(m p) t -> p m t", p=128), [oT_d])
                S.ld(gst, gst[:], gs_d.h[:, c0:c0 + 512].rearrange("(m p) t -> p m t", p=128), [gs_d])
                S.tt("pool", x2[:], yt[:], yt[:], ALU.mult, [yt], [x2])
                S.ts("dve", x2[:], x2[:], 0.044715, 1.0, ALU.mult, ALU.add, [x2], [x2])
                S.tt("pool", x2[:], x2[:], yt[:], ALU.mult, [x2, yt], [x2])
                S.act(x2[:], x2[:], AF.Sigmoid, [x2], [x2], scale=1.5957691216057308)
                S.tt("dve", sT[:], x2[:], yt[:], ALU.mult, [x2, yt], [sT])
                for mo in range(4):
                    p = pA[mo % 2]
                    for kc in range(4):
                        S.mm(p[:], wgl[:, kc, mo * 128:(mo + 1) * 128], sT[:, kc, :], kc == 0, kc == 3, [wgl, sT], [p])
                    S.act(sig[:], p[:], AF.Sigmoid, [p, bgl], [sig], bias=bgl[:, mo:mo + 1])
                    S.tt("dve", s2T[:, mo, :], sT[:, mo, :], sig[:], ALU.mult, [sT, sig], [s2T])
                for mo in range(8):
                    pa, pb = pA[mo % 2], pB[mo % 2]
                    for kc in range(4):
                        S.mm(pa[:], wss[:, kc, mo * 128:(mo + 1) * 128], s2T[:, kc, :], kc == 0, kc == 3, [wss, s2T], [pa])
                    for kc in range(4):
                        S.mm(pb[:], wat[:, kc, mo * 128:(mo + 1) * 128], ot_[:, kc, :], kc == 0, kc == 3, [wat, ot_], [pb])
                    S.tt("dve", ta[:], pa[:], gst[:, mo, :], ALU.mult, [pa, gst], [ta])
                    S.tt("dve", tb2[:], pb[:], gst[:, 8 + mo, :], ALU.mult, [pb, gst], [tb2])
                    S.tt("pool", mT[:, mo, :], ta[:], tb2[:], ALU.add, [ta, tb2], [mT])
                for sub in range(4):
                    r0 = c0 + sub * 128
                    x_, m_, s_ = xt[sub % 2], xm[sub % 2], ss[sub % 2]
                    S.ld(x_, x_[:], X.xs.h[r0:r0 + 128, :], [X.xs])
                    for hf in range(2):
                        p = pX[hf]
                        for kc in range(8):
                            S.mm(p[:], mT[:, kc, sub * 128:(sub + 1) * 128], wou[:, kc, hf * 512:(hf + 1) * 512], kc == 0, kc == 7, [mT, wou], [p])
                        S.tt("dve", m_[:, hf * 512:(hf + 1) * 512], p[:], g1r[:, hf * 512:(hf + 1) * 512], ALU.mult, [p, g1r], [m_])
                    S.tt("pool", m_[:], m_[:], x_[:], ALU.add, [m_, x_], [m_])
                    S.st(m_, xmix_d.h[r0:r0 + 128, :], m_[:], [xmix_d])
                    S.act(sq[:], m_[:], AF.Square, [m_], [sq, s_], accum=s_[:])
                    S.ts("dve", s_[:], s_[:], 1.0 / D, EPS, ALU.mult, ALU.add, [s_], [s_])
                    S.act(s_[:], s_[:], AF.Sqrt, [s_], [s_])
                    S.op("dve", lambda e, s_=s_: e.reciprocal(out=s_[:], in_=s_[:]), [s_], [s_])
                    S.ts("dve", xn[:], m_[:], s_[:, 0:1], None, ALU.mult, None, [m_, s_], [xn])
                    hb = h2b[sub % 2]
                    for kc in range(8):
                        p = pX[kc % 2]
                        S.tr(p[:, 0:128], xn[:, kc * 128:(kc + 1) * 128], ident[:], [xn, ident], [p])
                        S.ts("dve", h2f[:, kc, :], p[:, 0:128], G2[:, kc:kc + 1], modT[:, 24 + kc, 0:1], ALU.mult, ALU.add, [p, G2, modT], [h2f])
                    S.cp("pool", hb[:], h2f[:], [h2f], [hb])
                    S.st(hb, h2T_d.h[:, r0:r0 + 128].rearrange("(m p) t -> p m t", p=128), hb[:], [h2T_d])
                    for kc in range(8):
                        S.mm(pL[:, 0:32], h2f[:, kc, :], rwt[:, kc, :], kc == 0, kc == 7, [h2f, rwt], [pL])
                    S.tt("dve", lg[:], pL[:, 0:32], rbt[:], ALU.add, [pL, rbt], [lg])
                    S.op("dve", lambda e: e.max(out=t8[:], in_=lg[:]), [lg], [t8])
                    S.ts("dve", msk[:], lg[:], t8[:, 3:4], None, ALU.is_ge, None, [lg, t8], [msk])
                    S.ts("dve", sm[:], t8[:, 0:1], -1.0, None, ALU.mult, None, [t8], [sm])
                    S.act(ex[:], lg[:], AF.Exp, [lg, sm], [ex], bias=sm[:, 0:1])
                    S.tt("dve", ex[:], ex[:], msk[:], ALU.mult, [ex, msk], [ex])
                    S.op("dve", lambda e: e.tensor_reduce(out=sm[:], in_=ex[:], axis=AX.X, op=ALU.add), [ex], [sm])
                    S.op("dve", lambda e: e.reciprocal(out=sm[:], in_=sm[:]), [sm], [sm])
                    S.ts("dve", ex[:], ex[:], sm[:, 0:1], None, ALU.mult, None, [ex, sm], [ex])
                    S.tr(pL[0:32, 128:256], ex[:], ident[:], [ex, ident], [pL])
                    S.cp("dve", gsb[:], pL[0:32, 128:256], [pL], [gsb])
                    S.st(gsb, gT_d.h[:, r0:r0 + 128], gsb[:], [gT_d])
            if debug == 4:
                S.ld(xn, xn[:], xmix_d.h[0:128, :], [xmix_d])
                S.st(xn, dbg.h[:, :], xn[:], [dbg])
            S.end_phase()
        if stop == 4:
            return nc
        with S.phase():
            dbgm = 2 <= debug < 10
            NTH, NSTT = (1, 1) if dbgm else (4, 2)
            h2 = S.sbuf("h2", [128, 8, 1024], BF16)
            acc = S.sbuf("acc", [128, 8, D], F32)
            Wg = [S.sbuf("Wg", [128, 8, 256], BF16) for _ in range(8)]
            Wdn = [S.sbuf("Wdn", [128, D], BF16) for _ in range(8)]
            sg_ = [S.sbuf("sgst", [128, 8, 128], F32) for _ in range(2)]
            sd_ = [S.sbuf("sdst", [128, D], F32) for _ in range(2)]
            aT = [S.sbuf("aT", [128, 8, 512], BF16) for _ in range(2)]
            bgt = [S.sbuf("bgt", [128, 16], F32) for _ in range(2)]
            Ge = [S.sbuf("Ge", [128, 512], F32) for _ in range(2)]
            g_ = S.sbuf("g_", [128, 512], F32); u_ = S.sbuf("u_", [128, 512], F32); sgm = S.sbuf("sgm", [128, 512], F32)
            pG = [S.psum("pG", [128, 512], F32) for _ in range(2)]; pU = [S.psum("pU", [128, 512], F32) for _ in range(2)]
            pD = [S.psum("pD", [128, 512], F32) for _ in range(2)]
            g2r = S.sbuf("g2r", [128, D], F32); fgr = S.sbuf("fgr", [128, D], F32)
            S.ld(g2r, g2r[:], _bc(modrow_d.h[8:16, :].rearrange("a b -> (a b)").unsqueeze(0)), [modrow_d])
            S.ld(fgr, fgr[:], _bc(X.fg_d.h[0:1, :]), [X.fg_d])
            bds = S.sbuf("bds", [32, D], F32); S.ld(bds, bds[:], X.bdn.h[:, :], [X.bdn])
            gtt = S.sbuf("gtt", [32, 128], F32)
            ssf = S.sbuf("ssf", [128, 1], F32); sqf = S.sbuf("sqf", [128, D], BF16)
            ldc = [0]

            def load_g(e, j):
                for hf_ in range(2):
                    stg_ = sg_[ldc[0] % 2]; ldc[0] += 1
                    c_ = hf_ * 1024 + j * 128
                    S.ld(stg_, stg_[:], WGU(e, 0, D, c_, c_ + 128).rearrange("(k p) c -> p k c", p=128), [wgu_all if gather else X.wgu_d])
                    S.cp("act" if hf_ else "pool", Wg[j][:, :, hf_ * 128:(hf_ + 1) * 128], stg_[:], [stg_], [Wg[j]])

            def load_d(e, j):
                stg_ = sd_[ldc[0] % 2]; ldc[0] += 1
                S.ld(stg_, stg_[:], WD(e, j * 128, (j + 1) * 128, 0, D), [wd_all if gather else X.wd_d])
                S.cp("pool" if j % 2 else "act", Wdn[j][:], stg_[:], [stg_], [Wdn[j]])

            for th in range(NTH):
                t0 = th * 1024
                nld = NSTT * 512
                S.ld(h2, h2[:, :, 0:nld], h2T_d.h[:, t0:t0 + nld].rearrange("(m p) t -> p m t", p=128), [h2T_d])
                for j in range(8):
                    load_g(0, j)
                for j in range(8):
                    load_d(0, j)
                for e in range(32):
                    S.maybe_sync()
                    bg = bgt[e % 2]
                    S.ld(bg, bg[:], X.bguT.h[e, :, :], [X.bguT])
                    for st_ in range(NSTT):
                        c0 = st_ * 512
                        ge = Ge[(e * NSTT + st_) % 2]
                        S.ld(ge, ge[:], _bc(gT_d.h[e:e + 1, t0 + c0:t0 + c0 + 512]), [gT_d])
                        a_ = aT[(e * NSTT + st_) % 2]
                        for j in range(8):
                            pg, pu = pG[j % 2], pU[j % 2]
                            for kc in range(8):
                                S.mm(pg[:], Wg[j][:, kc, 0:128], h2[:, kc, c0:c0 + 512], kc == 0, kc == 7, [Wg[j], h2], [pg])
                            for kc in range(8):
                                S.mm(pu[:], Wg[j][:, kc, 128:256], h2[:, kc, c0:c0 + 512], kc == 0, kc == 7, [Wg[j], h2], [pu])
                            S.ts("dve", g_[:], pg[:], bg[:, j:j + 1], 7.0, ALU.add, ALU.min, [pg, bg], [g_])
                            S.act(sgm[:], g_[:], AF.Sigmoid, [g_], [sgm], scale=1.702)
                            S.ts("dve", u_[:], pu[:], bg[:, 8 + j:9 + j], 7.0, ALU.add, ALU.min, [pu, bg], [u_])
                            S.ts("pool", u_[:], u_[:], -7.0, 1.0, ALU.max, ALU.add, [u_], [u_])
                            S.tt("pool", g_[:], g_[:], sgm[:], ALU.mult, [g_, sgm], [g_])
                            S.tt("pool", g_[:], g_[:], u_[:], ALU.mult, [g_, u_], [g_])
                            S.tt("dve", a_[:, j, :], g_[:], ge[:], ALU.mult, [g_, ge], [a_])
                            if st_ == NSTT - 1 and e + 1 < 32:
                                load_g(e + 1, j)
                        for sub in range(4):
                            for hf in range(2):
                                pd = pD[(sub * 2 + hf) % 2]
                                for j in range(8):
                                    S.mm(pd[:], a_[:, j, sub * 128:(sub + 1) * 128], Wdn[j][:, hf * 512:(hf + 1) * 512], j == 0, j == 7, [a_, Wdn[j]], [pd])
                                av = acc[:, st_ * 4 + sub, hf * 512:(hf + 1) * 512]
                                if e == 0:
                                    S.cp("dve", av, pd[:], [pd], [acc])
                                else:
                                    S.tt("dve", av, av, pd[:], ALU.add, [acc, pd], [acc])
                    if e + 1 < 32:
                        for j in range(8):
                            load_d(e + 1, j)
                for ti in range(4 * NSTT):
                    r0 = t0 + ti * 128
                    S.ld(gtt, gtt[:], gT_d.h[:, r0:r0 + 128], [gT_d])
                    xm_ = sd_[ti % 2]
                    S.ld(xm_, xm_[:], xmix_d.h[r0:r0 + 128, :], [xmix_d])
                    for hf in range(2):
                        pd = pD[hf]
                        S.mm(pd[:], gtt[:], bds[:, hf * 512:(hf + 1) * 512], True, True, [gtt, bds], [pd])
                        av = acc[:, ti, hf * 512:(hf + 1) * 512]
                        S.tt("dve", av, av, pd[:], ALU.add, [acc, pd], [acc])
                    S.tt("pool", acc[:, ti, :], acc[:, ti, :], g2r[:], ALU.mult, [acc, g2r], [acc])
                    S.tt("dve", acc[:, ti, :], acc[:, ti, :], xm_[:], ALU.add, [acc, xm_], [acc])
                    S.act(sqf[:], acc[:, ti, :], AF.Square, [acc], [sqf, ssf], accum=ssf[:])
                    S.ts("dve", ssf[:], ssf[:], 1.0 / D, EPS, ALU.mult, ALU.add, [ssf], [ssf])
                    S.act(ssf[:], ssf[:], AF.Sqrt, [ssf], [ssf])
                    S.op("dve", lambda en: en.reciprocal(out=ssf[:], in_=ssf[:]), [ssf], [ssf])
                    S.stt(xm_[:], acc[:, ti, :], ssf[:, 0:1], fgr[:], ALU.mult, ALU.mult, [acc, ssf, fgr], [xm_])
                    S.st(xm_, out_d.h[r0:r0 + 128, :], xm_[:], [out_d])
            S.end_phase()
    return nc


_NC_CACHE = {}


def _rope_tables(pos):
    half = 32
    inv = (10000.0 ** (-np.arange(0, half, 2, dtype=np.float32) / half)).astype(np.float32)
    row = (pos // 64).astype(np.float32)
    col = (pos % 64).astype(np.float32)
    ang = np.concatenate([row[:, None] * inv, col[:, None] * inv], axis=-1).astype(np.float32)
    return np.cos(ang).astype(np.float32), np.sin(ang).astype(np.float32)


def prep_inputs(inp, gather=True):
    f = lambda a: np.ascontiguousarray(np.asarray(a, dtype=np.float32))
    x, c, ctx, c_ctx = f(inp["x"]), f(inp["c"]), f(inp["ctx"]), f(inp["c_ctx"])
    colT = lambda v, n: f(np.asarray(v, np.float32).reshape(n, 128).T)
    shared = {
        "w_mod": f(inp["w_mod"][0]),
        "b_modT": colT(inp["b_mod"][0], 48),
        "n1T": colT(inp["norm1_g"][0], 8),
        "n2T": colT(inp["norm2_g"][0], 8),
        "w_in": f(inp["w_in"][0]),
        "ident": np.eye(128, dtype=np.float32),
        "qg": f(inp["q_norm_g"][0]).reshape(1, 64),
        "kg": f(inp["k_norm_g"][0]).reshape(1, 64),
        "rmask": f((np.arange(128)[:, None] // 16 == np.arange(8)[None, :])),
        "dskipT": colT(inp["s5_d"][0], 4),
        "w_glu": f(inp["w_glu"][0]), "b_gluT": colT(inp["b_glu"][0], 4),
        "w_ssm": f(inp["w_ssm_out"][0]), "w_att": f(inp["w_attn_out"][0]), "w_out": f(inp["w_out"][0]),
        "rw": f(inp["router_w"][0]), "rb": f(inp["router_b"][0]).reshape(1, 32),

        "bguT": f(np.asarray(inp["b_gate_up"][0], np.float32).reshape(32, 16, 128).transpose(0, 2, 1)),
        "bdn": f(inp["b_down"][0]), "fg": f(inp["final_norm_g"]).reshape(1, D),
    }
    rep = np.zeros((8, 64, 128), np.float32)
    for d_ in range(2):
        for ct in range(4):
            for gl in range(8):
                rep[d_ * 4 + ct, d_ * 32 + ct * 8 + gl, gl * 16:(gl + 1) * 16] = 1.0
    shared["rep"] = rep
    maps = []
    for core in range(8):
        b, half = core // 2, core % 2
        pos = np.arange(L)
        if half == 0:
            xs_, cs_ = x[b], ctx[b]
        else:
            xs_, cs_ = x[b][::-1], ctx[b][::-1]
            pos = pos[::-1]
        rc_, rs_ = _rope_tables(pos)
        m = dict(shared)
        do = [0, 1] if half == 0 else [1, 0]
        g5 = lambda k: np.asarray(inp[k][0], np.float32)[do]
        m["lam_re"] = f(g5("s5_lam_re").reshape(64, 64))
        m["lam_im"] = f(g5("s5_lam_im").reshape(64, 64))
        m["lstep"] = f(g5("s5_log_step").reshape(64, 1))
        tb_ = lambda a: f(a.reshape(2, 4, 8, 64, 16).transpose(0, 1, 2, 4, 3).reshape(8, 128, 64))
        m["bT_re"], m["bT_im"] = tb_(g5("s5_b_re")), tb_(g5("s5_b_im"))
        tc_ = lambda a: a.reshape(2, 4, 8, 16, 64).transpose(0, 1, 4, 2, 3).reshape(8, 64, 128)
        cr_, ci_ = tc_(g5("s5_c_re")), tc_(g5("s5_c_im"))
        m["cst_a"] = f(np.concatenate([cr_, ci_], axis=1))
        m["cst_b"] = f(np.concatenate([ci_, cr_], axis=1))
        if gather:
            m["wgu_sh"] = f(inp["w_gate_up"][0][4 * core:4 * core + 4]).reshape(4 * D, 2048)
            m["wd_sh"] = f(inp["w_down"][0][4 * core:4 * core + 4]).reshape(4 * D, D)
        else:
            m["wgu"], m["wd"] = f(inp["w_gate_up"][0]), f(inp["w_down"][0])
        m.update({"xs": f(xs_), "ctxs": f(cs_), "cvec": f(np.stack([c[b], c_ctx], axis=1).reshape(8, 128, 2).transpose(1, 0, 2)),
                  "ropec": f(rc_), "ropes": f(rs_)})
        maps.append(m)
    return maps


def kernel(**inputs):
    maps = prep_inputs(inputs, gather=False)
    if "nc" not in _NC_CACHE:
        _NC_CACHE["nc"] = build(debug=False, gather=False)
    res = run_bass_kernel_spmd(_NC_CACHE["nc"], maps, core_ids=list(range(8)))
    out = np.zeros((4, L, D), np.float32)
    for core in range(8):
        b, half = core // 2, core % 2
        y = np.asarray(res.results[core]["out"], np.float32)
        if half == 0:
            out[b, :NOWN] = y
        else:
            out[b, NOWN:] = y[::-1]
    return out
```

```python
import os
import numpy as np
from contextlib import ExitStack
import concourse.bass as bass
import concourse.mybir as mybir
from concourse.bass_utils import run_bass_kernel_spmd

F32 = mybir.dt.float32
BF16 = mybir.dt.bfloat16
AF = mybir.ActivationFunctionType
ALU = mybir.AluOpType
AX = mybir.AxisListType

ENGS = ("pe", "act", "dve", "pool", "sp")
D = 1024
L = 8192
LC = 256
NOWN = 4096
NALL = LC + L
DIN = 3328
EPS = 1e-6


_ALL_BUFS = []


class Buf:
    def __init__(self, name, h):
        _ALL_BUFS.append(self)
        self.name = name
        self.h = h
        self.last_w = None
        self.readers = []
        self.dma_sem = None
        self.dma_cnt = 0

    def __getitem__(self, idx):
        return self.h[idx]


class Sched:
    def __init__(self, nc, stack):
        self.nc = nc
        self.stack = stack
        self.sems = {}
        self.free_sems = []
        self.used_sems = []
        self.nsem = 0
        self.epoch = {e: 0 for e in ENGS}
        for e in ENGS:
            self.sems[(e, 0)] = self._new_sem()
        self.cnt = {e: 0 for e in ENGS}
        self.prog = {e: [] for e in ENGS}
        self.waited = {e: {} for e in ENGS}
        self.nbuf = 0
        self.sb = {}
        self.dma_keys = {}
        self.ph = None

    def _new_sem(self):
        if self.free_sems:
            sm = self.free_sems.pop()
        else:
            self.nsem += 1
            sm = self.stack.enter_context(self.nc.semaphore(f"sem{self.nsem}"))
        self.used_sems.append(sm)
        return sm

    def phase(self):
        self.ph = ExitStack()
        return self.ph

    SB_LIMIT = 172 * 1024

    def sbuf(self, name, shape, dt):
        nb = int(np.prod(shape[1:])) * (4 if dt == F32 else 2)
        nb = (nb + 31) // 32 * 32
        key = "p" if self.ph is self.stack else "t"
        self.sb[key] = self.sb.get(key, 0) + nb
        assert self.sb.get("p", 0) + self.sb.get("t", 0) <= self.SB_LIMIT, (name, self.sb)
        self.nbuf += 1
        h = self.ph.enter_context(self.nc.sbuf_tensor(f"{name}_{self.nbuf}", list(shape), dt))
        return Buf(name, h)

    def psum(self, name, shape, dt):
        self.nbuf += 1
        h = self.ph.enter_context(self.nc.psum_tensor(f"{name}_{self.nbuf}", list(shape), dt))
        return Buf(name, h)

    def dram(self, name, shape, dt, kind="Internal"):
        t = self.nc.dram_tensor(name, list(shape), dt, kind=kind)
        return Buf(name, t.ap())

    def _dma_sem(self, buf):
        if buf.dma_sem is None:
            self.nbuf += 1
            buf.dma_sem = self._new_sem()
            buf.dma_key = ("d", self.nbuf)
            buf.dma_cnt = 0
            self.sems[buf.dma_key] = buf.dma_sem
        return buf.dma_sem

    def _collect(self, eng, reads, writes):
        need = {}

        def add(ev):
            if ev is None:
                return
            k, v = ev
            if need.get(k, 0) < v:
                need[k] = v
        for b in reads:
            add(b.last_w)
        for b in writes:
            add(b.last_w)
            for ev in b.readers:
                add(ev)
        waits = []
        for k, v in need.items():
            if self.waited[eng].get(k, 0) >= v:
                continue
            self.waited[eng][k] = v
            waits.append((k, v))
        return waits

    def _commit(self, ev, reads, writes):
        for b in writes:
            b.last_w = ev
            b.readers = []
        for b in reads:
            if b not in writes:
                b.readers.append(ev)
                if len(b.readers) > 16:
                    best = {}
                    for k, v in b.readers:
                        if best.get(k, 0) < v:
                            best[k] = v
                    b.readers = list(best.items())

    LIMIT = 3000

    def _roll(self, eng):
        if self.cnt[eng] >= self.EPOCH:
            self.epoch[eng] += 1
            self.cnt[eng] = 0
            k = (eng, self.epoch[eng])
            self.sems[k] = self._new_sem()

    def op(self, eng, fn, reads=(), writes=()):
        assert self.cnt[eng] < 4000, "semaphore count too large: add maybe_sync()"
        waits = self._collect(eng, reads, writes)
        self.cnt[eng] += 1
        key = (eng, self.epoch[eng])
        ev = (key, self.cnt[eng])
        self.prog[eng].append((waits, fn, (key, 1)))
        self._commit(ev, reads, writes)
        return ev

    def dma(self, q, fns, sem_buf, reads=(), writes=()):
        assert sem_buf.dma_cnt < 4000, "dma semaphore count too large: add maybe_sync()"
        self._dma_sem(sem_buf)
        key = sem_buf.dma_key
        waits = self._collect(q, reads, writes)
        for i, fn in enumerate(fns):
            sem_buf.dma_cnt += 16
            self.prog[q].append((waits if i == 0 else [], fn, (key, 16)))
        ev = (key, sem_buf.dma_cnt)
        self.dma_keys[key] = sem_buf.dma_cnt
        self._commit(ev, reads, writes)
        return ev

    def barrier(self):
        evs = [((e, self.epoch[e]), self.cnt[e]) for e in ENGS if self.cnt[e] > 0]
        evs += list(self.dma_keys.items())
        for e in ENGS:
            waits = []
            for k, v in evs:
                if k == (e, self.epoch[e]) or self.waited[e].get(k, 0) >= v:
                    continue
                self.waited[e][k] = v
                waits.append((k, v))
            if waits:
                self.prog[e].append((waits, None, None))

    def emit(self):
        nc = self.nc
        with nc.Block() as block:
            deco = {"pe": block.tensor, "act": block.scalar, "dve": block.vector,
                    "pool": block.gpsimd, "sp": block.sync}
            for e in ENGS:
                prog = self.prog[e]
                if not prog:
                    continue

                def body(engine, prog=prog):
                    for waits, fn, inc in prog:
                        for k, v in waits:
                            engine.wait_ge(self.sems[k], v)
                        if fn is None:
                            continue
                        fn(engine).then_inc(self.sems[inc[0]], inc[1])

                deco[e](body)
        self.prog = {e: [] for e in ENGS}

    def sync_all(self):
        if os.environ.get("SYNCDBG"):
            print("SYNC", getattr(self, "nsync", 0), dict(self.cnt), {e: len(self.prog[e]) for e in ENGS}, flush=True)
        self.barrier()
        if not hasattr(self, "hs_pairs"):
            self.hs_pairs = [(self.stack.enter_context(self.nc.semaphore(f"hs{i}")), self.stack.enter_context(self.nc.semaphore(f"go{i}"))) for i in range(2)]
            self.nsync = 0
        pair = self.hs_pairs[self.nsync % 2]
        other = self.hs_pairs[(self.nsync + 1) % 2]
        self.nsync += 1
        used = list(self.used_sems) + list(other)
        for e in ENGS:
            if e == "sp":
                continue
            self.prog[e].append(([], lambda en: en.nop(), ("__hs", 1)))
            self.prog[e].append(([("__go", 1)], None, None))
        self.sems["__hs"], self.sems["__go"] = pair

        def clr(en):
            for sm in used:
                en.sem_clear(sm)
            return en.nop()
        self.prog["sp"].append(([("__hs", 4)], clr, ("__go", 1)))
        self.emit()
        self.free_sems.extend(self.used_sems)
        self.used_sems = []
        self.sems = {}
        self.dma_keys = {}
        self.waited = {e: {} for e in ENGS}
        for e in ENGS:
            self.epoch[e] += 1
            self.cnt[e] = 0
            self.sems[(e, self.epoch[e])] = self._new_sem()
        for b in _ALL_BUFS:
            b.last_w = None
            b.readers = []
            b.dma_sem = None
            b.dma_cnt = 0

    def maybe_sync(self, lim=1500):
        m = max(max(self.cnt.values()), max([b.dma_cnt for b in _ALL_BUFS] + [0]))
        if m >= lim:
            self.sync_all()

    def end_phase(self):
        self.sync_all()
        self.ph.close()
        self.ph = None
        self.sb["t"] = 0

    def mm(self, out, lhsT, rhs, start, stop, R, W):
        return self.op("pe", lambda e: e.matmul(out, lhsT, rhs, start=start, stop=stop), R, W)

    def tr(self, out, in_, ident, R, W):
        return self.op("pe", lambda e: e.transpose(out, in_, ident), R, W)

    def act(self, out, in_, func, R, W, bias=None, scale=None, accum=None):
        kw = {}
        if bias is not None:
            kw["bias"] = bias
        if scale is not None:
            kw["scale"] = scale
        if accum is not None:
            kw["accum_out"] = accum
        return self.op("act", lambda e: e.activation(out=out, in_=in_, func=func, **kw), R, W)

    def tt(self, eng, out, a, b, op, R, W):
        return self.op(eng, lambda e: e.tensor_tensor(out=out, in0=a, in1=b, op=op), R, W)

    def ts(self, eng, out, a, s1, s2, op0, op1, R, W):
        if op1 is None:
            return self.op(eng, lambda e: e.tensor_scalar(out=out, in0=a, scalar1=s1, scalar2=None, op0=op0), R, W)
        return self.op(eng, lambda e: e.tensor_scalar(out=out, in0=a, scalar1=s1, scalar2=s2, op0=op0, op1=op1), R, W)

    def stt(self, out, a, s, b, op0, op1, R, W):
        return self.op("dve", lambda e: e.scalar_tensor_tensor(out=out, in0=a, scalar=s, in1=b, op0=op0, op1=op1), R, W)

    def cp(self, eng, out, in_, R, W):
        if eng == "act" or (out.dtype == in_.dtype and out.dtype == BF16):
            return self.op("act", lambda e: e.activation(out=out, in_=in_, func=AF.Identity), R, W)
        if out.dtype == in_.dtype:
            return self.op(eng, lambda e: e.tensor_scalar(out=out, in0=in_, scalar1=1.0, scalar2=None, op0=ALU.mult), R, W)
        return self.op(eng, lambda e: e.tensor_copy(out=out, in_=in_), R, W)

    def ld(self, buf, out, in_, R, q="sp", W=None):
        return self.dma(q, [lambda e: e.dma_start(out=out, in_=in_)], buf, reads=R, writes=[buf] if W is None else W)

    def st(self, buf, out, in_, W, q="sp", R=None):
        return self.dma(q, [lambda e: e.dma_start(out=out, in_=in_)], buf, reads=[buf] if R is None else R, writes=W)


def _bc(ap, n=128):
    return ap.partition_broadcast(n)


def build(debug=False, stop=99, gather=True):
    nc = bass.Bass("TRN2", target_bir_lowering=False)
    del _ALL_BUFS[:]
    with ExitStack() as st:
        st.enter_context(nc.allow_non_contiguous_dma(reason="small parameter layouts"))
        S = Sched(nc, st)
        _IN = {
            "xs": ("xs", [L, D]),
            "ctxs": ("ctxs", [LC, D]),
            "cvec": ("cvec", [128, 8, 2]),
            "w_mod": ("w_mod", [D, 6 * D]),
            "b_modT": ("b_modT", [128, 48]),
            "n1T": ("n1T", [128, 8]),
            "n2T": ("n2T", [128, 8]),
            "w_in": ("w_in", [D, DIN]),
            "ident_d": ("ident", [128, 128]),
            "qg": ("qg", [1, 64]),
            "kg": ("kg", [1, 64]),
            "ropec": ("ropec", [L, 32]),
            "ropes": ("ropes", [L, 32]),
            "lam_re": ("lam_re", [64, 64]),
            "lam_im": ("lam_im", [64, 64]),
            "lstep": ("lstep", [64, 1]),
            "bT_re": ("bT_re", [8, 128, 64]),
            "bT_im": ("bT_im", [8, 128, 64]),
            "cst_a": ("cst_a", [8, 128, 128]),
            "cst_b": ("cst_b", [8, 128, 128]),
            "rep_d": ("rep", [8, 64, 128]),
            "rmask_d": ("rmask", [128, 8]),
            "dskipT": ("dskipT", [128, 4]),
            "w_glu": ("w_glu", [512, 512]),
            "b_gluT": ("b_gluT", [128, 4]),
            "w_ssm": ("w_ssm", [512, D]),
            "w_att": ("w_att", [512, D]),
            "w_out": ("w_out", [D, D]),
            "rw_d": ("rw", [D, 32]),
            "rb_d": ("rb", [1, 32]),
            "wgu_d": ("wgu", [32, D, 2048]),
            "wd_d": ("wd", [32, D, D]),
            "wgu_sh": ("wgu_sh", [4 * D, 2048]),
            "wd_sh": ("wd_sh", [4 * D, D]),
            "bguT": ("bguT", [32, 128, 16]),
            "bdn": ("bdn", [32, D]),
            "fg_d": ("fg", [1, D]),
        }

        class _Lazy:
            def __init__(self):
                self.c = {}

            def __getattr__(self, k):
                if k not in self.c:
                    n_, sh_ = _IN[k]
                    self.c[k] = S.dram(n_, sh_, F32, kind="ExternalInput")
                return self.c[k]
        X = _Lazy()
        out_d = S.dram("out", [NOWN, D], F32, kind="ExternalOutput")
        dbg = S.dram("dbg", [128, 1024], F32, kind="ExternalOutput") if debug else None
        uT_d = S.dram("uT_d", [512, NALL], BF16)
        gs_d = S.dram("gs_d", [2048, NOWN], BF16)
        modrow_d = S.dram("modrow_d", [16, 128], F32)

        S.ph = st
        ident = S.sbuf("ident", [128, 128], F32)
        identb = S.sbuf("identb", [128, 128], BF16)
        modT = S.sbuf("modT", [128, 48, 2], F32)
        G1 = S.sbuf("G1", [128, 8, 2], F32)
        G2 = S.sbuf("G2", [128, 8], F32)
        S.ph = None
        kt_d = S.dram("kt_d", [2, 128, NALL], BF16)
        va_d = S.dram("va_d", [128, 66 * 132], BF16)
        qt_d = S.dram("qt_d", [128, 4 * NOWN], BF16)
        yT_d = S.dram("yT_d", [512, NOWN], F32)
        oT_d = S.dram("oT_d", [512, NOWN], BF16)
        xmix_d = S.dram("xmix_d", [NOWN, D], F32)
        h2T_d = S.dram("h2T_d", [D, NOWN], BF16)
        gT_d = S.dram("gT_d", [32, NOWN], F32)

        if gather:
            wgu_b = S.dram("wgu_b", [4 * D, 2048], F32); wd_b = S.dram("wd_b", [4 * D, D], F32)
            wgu_all = S.dram("wgu_all", [32 * D, 2048], F32); wd_all = S.dram("wd_all", [32 * D, D], F32)
            WGU = lambda e, r0, r1, c0, c1: wgu_all.h[e * D + r0:e * D + r1, c0:c1]
            WD = lambda e, r0, r1, c0, c1: wd_all.h[e * D + r0:e * D + r1, c0:c1]
            wgu_src, wd_src = wgu_all, wd_all
        else:
            WGU = lambda e, r0, r1, c0, c1: X.wgu_d.h[e, r0:r1, c0:c1]
            WD = lambda e, r0, r1, c0, c1: X.wd_d.h[e, r0:r1, c0:c1]

        with S.phase():
            if gather:
                for e4 in range(4):
                    S.dma("pool", [lambda en, e4=e4: en.dma_start(out=wgu_b.h[e4 * D:(e4 + 1) * D, :], in_=X.wgu_sh.h[e4 * D:(e4 + 1) * D, :])], wgu_b, reads=[X.wgu_sh], writes=[wgu_b])
                    S.dma("pool", [lambda en, e4=e4: en.dma_start(out=wd_b.h[e4 * D:(e4 + 1) * D, :], in_=X.wd_sh.h[e4 * D:(e4 + 1) * D, :])], wd_b, reads=[X.wd_sh], writes=[wd_b])
                rg = [list(range(8))]
                S.dma("pool", [lambda en: en.collective_compute("AllGather", ALU.bypass, replica_groups=rg, ins=[wgu_b.h.opt()], outs=[wgu_all.h.opt()])], wgu_all, reads=[wgu_b], writes=[wgu_all])
                S.dma("pool", [lambda en: en.collective_compute("AllGather", ALU.bypass, replica_groups=rg, ins=[wd_b.h.opt()], outs=[wd_all.h.opt()])], wd_all, reads=[wd_b], writes=[wd_all])
            cv = S.sbuf("cv", [128, 8, 2], F32)
            sg = S.sbuf("sg", [128, 8, 2], F32)
            bm = S.sbuf("bm", [128, 48], F32)
            n1 = S.sbuf("n1", [128, 8], F32)
            n2 = S.sbuf("n2", [128, 8], F32)
            S.ld(ident, ident[:], X.ident_d.h[:, :], [X.ident_d])
            S.cp("dve", identb[:], ident[:], [ident], [identb])
            S.ld(cv, cv[:], X.cvec.h[:, :, :], [X.cvec])
            S.ld(bm, bm[:], X.b_modT.h[:, :], [X.b_modT])
            S.ld(n1, n1[:], X.n1T.h[:, :], [X.n1T])
            S.ld(n2, n2[:], X.n2T.h[:, :], [X.n2T])
            S.act(sg[:], cv[:], AF.Sigmoid, [cv], [sg])
            S.tt("dve", cv[:], cv[:], sg[:], ALU.mult, [cv, sg], [cv])
            wm = [S.sbuf("wm", [128, 8, 512], F32) for _ in range(2)]
            pm = S.psum("pm", [128, 48, 2], F32)
            for blk in range(12):
                w = wm[blk % 2]
                S.ld(w, w[:], X.w_mod.h[:, blk * 512:(blk + 1) * 512].rearrange("(k p) c -> p k c", p=128), [X.w_mod])
                for mi in range(4):
                    m = blk * 4 + mi
                    for kc in range(8):
                        S.mm(pm[:, m, :], w[:, kc, mi * 128:(mi + 1) * 128], cv[:, kc, :], kc == 0, kc == 7, [w, cv], [pm])
            for j in range(2):
                S.tt("dve", modT[:, :, j], pm[:, :, j], bm[:], ALU.add, [pm, bm], [modT])
            for j in range(2):
                S.stt(G1[:, :, j], modT[:, 8:16, j], 1.0, n1[:], ALU.add, ALU.mult, [modT, n1], [G1])
            S.stt(G2[:], modT[:, 32:40, 0], 1.0, n2[:], ALU.add, ALU.mult, [modT, n2], [G2])
            gcol = S.sbuf("gcol", [128, 16], F32)
            S.cp("dve", gcol[:, 0:8], modT[:, 16:24, 0], [modT], [gcol])
            S.cp("dve", gcol[:, 8:16], modT[:, 40:48, 0], [modT], [gcol])
            pg = S.psum("pg", [16, 128], F32)
            S.tr(pg[:], gcol[:], ident[:], [gcol, ident], [pg])
            grow = S.sbuf("grow", [16, 128], F32)
            S.cp("dve", grow[:], pg[:], [pg], [grow])
            S.st(grow, modrow_d.h[:, :], grow[:], [modrow_d])
            if debug == 10:
                dt_ = S.sbuf("dt_", [128, 1024], F32)
                S.op("pool", lambda e: e.memset(dt_[:], 0.0), (), [dt_])
                S.cp("dve", dt_[:, 0:96], modT[:].rearrange("p a b -> p (a b)"), [modT], [dt_])
                S.st(dt_, dbg.h[:, :], dt_[:], [dbg])
            S.end_phase()
        if debug == 10 or stop == 0:
            return nc

        with S.phase():
            ktt = [S.sbuf("ktt", [128, 2, 128], BF16) for _ in range(2)]
            vat = [S.sbuf("vat", [128, 2, 66], BF16) for _ in range(2)]
            qtt = [S.sbuf("qtt", [128, 4, 128], BF16) for _ in range(2)]
            wst = S.sbuf("wst", [128, 8, 104], F32)
            wbf = S.sbuf("wbf", [128, 8, DIN], BF16)
            for cb in range(32):
                S.ld(wst, wst[:], X.w_in.h[:, cb * 104:(cb + 1) * 104].rearrange("(k p) c -> p k c", p=128), [X.w_in])
                S.cp("pool" if cb % 2 else "act", wbf[:, :, cb * 104:(cb + 1) * 104], wst[:], [wst], [wbf])
            qgt = S.sbuf("qgt", [128, 64], F32)
            kgt = S.sbuf("kgt", [128, 64], F32)
            S.ld(qgt, qgt[:], _bc(X.qg.h[0:1, :]), [X.qg])
            S.ld(kgt, kgt[:], _bc(X.kg.h[0:1, :]), [X.kg])
            S.ts("dve", qgt[:], qgt[:], 0.125, None, ALU.mult, None, [qgt], [qgt])
            for v_ in vat:
                S.op("pool", lambda e, v_=v_: e.memset(v_[:], 1.0), (), [v_])
            xt = [S.sbuf("xt", [128, D], F32) for _ in range(2)]
            sq = S.sbuf("sq", [128, D], BF16)
            ss = [S.sbuf("ss", [128, 1], F32) for _ in range(2)]
            xn = [S.sbuf("xn", [128, D], BF16) for _ in range(2)]
            hT = [S.sbuf("hT", [128, 8, 512], BF16) for _ in range(2)]
            pT = [S.psum("pT", [128, D], BF16) for _ in range(1)]
            pf = [S.psum("pf", [128, 512], F32) for _ in range(2)]
            kvq = S.sbuf("kvq", [128, 6, 512], BF16)
            uo = [S.sbuf("uo", [128, 4, 512], BF16) for _ in range(1)]
            go = [S.sbuf("go", [128, 8, 512], BF16) for _ in range(1)]
            rc = [S.sbuf("rc", [128, 32], F32) for _ in range(2)]
            rs = [S.sbuf("rs", [128, 32], F32) for _ in range(2)]
            qk = [S.sbuf("qk", [128, 10, 64], F32) for _ in range(2)]
            qsq = S.sbuf("qsq", [128, 10, 64], F32)
            qss = [S.sbuf("qss", [128, 10], F32) for _ in range(2)]
            t1 = S.sbuf("t1", [128, 10, 32], F32)
            t2 = S.sbuf("t2", [128, 10, 32], F32)
            qr = [S.sbuf("qr", [128, 12, 64], BF16) for _ in range(2)]
            nsup = NALL // 512 + 1
            cnt = 0
            P1CUT = int(os.environ.get("P1CUT", "9"))
            for su in range(17 if P1CUT > 0 else 0):
                S.maybe_sync()
                ntile = 2 if su == 0 else 4
                tok0 = 0 if su == 0 else LC + (su - 1) * 512
                own = 1 <= su <= 8
                j = 1 if su == 0 else 0
                h = hT[su % 2]
                for sb_ in range(ntile):
                    x_ = xt[cnt % 2]
                    s_ = ss[cnt % 2]
                    n_ = xn[cnt % 2]
                    p_ = pT[0]
                    src = X.ctxs.h[sb_ * 128:(sb_ + 1) * 128, :] if su == 0 else X.xs.h[(su - 1) * 512 + sb_ * 128:(su - 1) * 512 + (sb_ + 1) * 128, :]
                    S.ld(x_, x_[:], src, [X.ctxs if su == 0 else X.xs])
                    S.act(sq[:], x_[:], AF.Square, [x_], [sq, s_], accum=s_[:])
                    S.ts("dve", s_[:], s_[:], 1.0 / D, EPS, ALU.mult, ALU.add, [s_], [s_])
                    S.act(s_[:], s_[:], AF.Sqrt, [s_], [s_])
                    S.op("dve", lambda e, s_=s_: e.reciprocal(out=s_[:], in_=s_[:]), [s_], [s_])
                    S.ts("dve", n_[:], x_[:], s_[:, 0:1], None, ALU.mult, None, [x_, s_], [n_])
                    for kc in range(8):
                        S.tr(p_[:, kc * 128:(kc + 1) * 128], n_[:, kc * 128:(kc + 1) * 128], identb[:], [n_, identb], [p_])
                    for kc in range(8):
                        o_ = h[:, kc, sb_ * 128:(sb_ + 1) * 128]
                        i_ = p_[:, kc * 128:(kc + 1) * 128]
                        if kc % 2 == 0:
                            S.act(o_, i_, AF.Identity, [p_, G1, modT], [h], bias=modT[:, kc, j:j + 1], scale=G1[:, kc, j:j + 1])
                        else:
                            S.ts("dve", o_, i_, G1[:, kc, j:j + 1], modT[:, kc, j:j + 1], ALU.mult, ALU.add, [p_, G1, modT], [h])
                    cnt += 1
                ncol = ntile * 128
                if P1CUT < 2:
                    continue
                u_ = uo[0]
                for mo in range(4):
                    p = pf[mo % 2]
                    for kc in range(8):
                        S.mm(p[:, 0:ncol], wbf[:, kc, mo * 128:(mo + 1) * 128], h[:, kc, 0:ncol], kc == 0, kc == 7, [wbf, h], [p])
                    S.cp("act" if mo % 2 else "dve", u_[:, mo, 0:ncol], p[:, 0:ncol], [p], [u_])
                S.st(u_, uT_d.h[:, tok0:tok0 + ncol].rearrange("(m p) t -> p m t", p=128), u_[:, :, 0:ncol], [uT_d])
                if own:
                    g_ = go[0]
                    o0 = (su - 1) * 512
                    for gh in range(2):
                        for mo in range(8):
                            p = pf[mo % 2]
                            c0 = 1280 + (gh * 8 + mo) * 128
                            for kc in range(8):
                                S.mm(p[:], wbf[:, kc, c0:c0 + 128], h[:, kc, :], kc == 0, kc == 7, [wbf, h], [p])
                            S.act(g_[:, mo, :], p[:], AF.Sigmoid, [p], [g_])
                        S.st(g_, gs_d.h[gh * 1024:(gh + 1) * 1024, o0:o0 + 512].rearrange("(m p) t -> p m t", p=128), g_[:], [gs_d])
                if P1CUT < 3:
                    continue
                ncq = 6 if own else 2
                for c in range(ncq):
                    p = pf[c % 2]
                    c0 = 512 + c * 128
                    for kc in range(8):
                        S.mm(p[:, 0:ncol], wbf[:, kc, c0:c0 + 128], h[:, kc, 0:ncol], kc == 0, kc == 7, [wbf, h], [p])
                    S.cp("act" if c % 2 else "dve", kvq[:, c, 0:ncol], p[:, 0:ncol], [p], [kvq])
                P1SUB = int(os.environ.get("P1SUB", "9"))
                for sb_ in range(ntile if P1SUB >= 2 else 0):
                    ti = (0 if su == 0 else 2 + (su - 1) * 4) + sb_
                    lat = su > 0
                    q_ = qk[ti % 2]
                    pt_ = pT[0]
                    for c in range(ncq):
                        S.tr(pt_[:, c * 128:(c + 1) * 128], kvq[:, c, sb_ * 128:(sb_ + 1) * 128], identb[:], [kvq, identb], [pt_])
                    if P1SUB < 3:
                        continue
                    S.cp("act", q_[:, 8:10, :], pt_[:, 0:128].rearrange("p (a b) -> p a b", b=64), [pt_], [q_])
                    if P1SUB < 4:
                        continue
                    va_ = vat[ti % 2]
                    S.cp("dve", va_[:, :, 0:64], pt_[:, 128:256].rearrange("p (a b) -> p a b", b=64), [pt_], [va_])
                    if P1SUB < 5:
                        continue
                    S.st(va_, va_d.h[:, ti * 132:(ti + 1) * 132], va_[:].rearrange("p a b -> p (a b)"), [va_d])
                    h0 = 8
                    if own:
                        S.cp("act", q_[:, 0:8, :], pt_[:, 256:768].rearrange("p (a b) -> p a b", b=64), [pt_], [q_])
                        h0 = 0
                    if P1CUT < 4:
                        continue
                    nh = 10 - h0
                    s2 = qss[ti % 2]
                    S.tt("dve", qsq[:, h0:10, :], q_[:, h0:10, :], q_[:, h0:10, :], ALU.mult, [q_], [qsq])
                    S.op("dve", lambda e, s2=s2, h0=h0: e.tensor_reduce(out=s2[:, h0:10], in_=qsq[:, h0:10, :], axis=AX.X, op=ALU.add), [qsq], [s2])
                    S.ts("dve", s2[:, h0:10], s2[:, h0:10], 1.0 / 64, EPS, ALU.mult, ALU.add, [s2], [s2])
                    S.act(s2[:, h0:10], s2[:, h0:10], AF.Sqrt, [s2], [s2])
                    S.op("dve", lambda e, s2=s2, h0=h0: e.reciprocal(out=s2[:, h0:10], in_=s2[:, h0:10]), [s2], [s2])
                    S.tt("dve", q_[:, h0:10, :], q_[:, h0:10, :], s2[:, h0:10].unsqueeze(2).to_broadcast([128, nh, 64]), ALU.mult, [q_, s2], [q_])
                    if own:
                        S.tt("dve", q_[:, 0:8, :], q_[:, 0:8, :], qgt[:].unsqueeze(1).to_broadcast([128, 8, 64]), ALU.mult, [q_, qgt], [q_])
                    S.tt("dve", q_[:, 8:10, :], q_[:, 8:10, :], kgt[:].unsqueeze(1).to_broadcast([128, 2, 64]), ALU.mult, [q_, kgt], [q_])
                    if P1CUT < 5:
                        continue
                    r_ = qr[ti % 2]
                    if lat:
                        c_ = rc[ti % 2]
                        sn = rs[ti % 2]
                        r0 = (su - 1) * 512 + sb_ * 128
                        S.ld(c_, c_[:], X.ropec.h[r0:r0 + 128, :], [X.ropec])
                        S.ld(sn, sn[:], X.ropes.h[r0:r0 + 128, :], [X.ropes])
                        ev_ = q_[:, h0:10, 0::2]
                        od_ = q_[:, h0:10, 1::2]
                        cb_ = c_[:].unsqueeze(1).to_broadcast([128, nh, 32])
                        sb2 = sn[:].unsqueeze(1).to_broadcast([128, nh, 32])
                        S.tt("dve", t1[:, h0:10, :], ev_, cb_, ALU.mult, [q_, c_], [t1])
                        S.tt("dve", t2[:, h0:10, :], od_, sb2, ALU.mult, [q_, sn], [t2])
                        S.tt("dve", t1[:, h0:10, :], t1[:, h0:10, :], t2[:, h0:10, :], ALU.subtract, [t1, t2], [t1])
                        S.tt("dve", t2[:, h0:10, :], ev_, sb2, ALU.mult, [q_, sn], [t2])
                        S.tt("dve", od_, od_, cb_, ALU.mult, [q_, c_], [q_])
                        S.tt("dve", od_, od_, t2[:, h0:10, :], ALU.add, [q_, t2], [q_])
                        S.cp("dve", ev_, t1[:, h0:10, :], [t1], [q_])
                    if own:
                        S.cp("act", r_[:, 0:8, :], q_[:, 0:8, :], [q_], [r_])
                    S.cp("dve", r_[:, 8:12:2, :], q_[:, 8:10, :], [q_], [r_])
                    S.cp("dve", r_[:, 9:12:2, :], q_[:, 8:10, :], [q_], [r_])
                    if P1CUT < 6:
                        continue
                    pt_ = pT[0]
                    kt_ = ktt[ti % 2]
                    for kv in range(2):
                        S.tr(pt_[:, kv * 128:(kv + 1) * 128], r_[:, 8 + 2 * kv:10 + 2 * kv, :].rearrange("p a b -> p (a b)"), identb[:], [r_, identb], [pt_])
                    S.cp("dve", kt_[:], pt_[:, 0:256].rearrange("p (a b) -> p a b", b=128), [pt_], [kt_])
                    S.st(kt_, kt_d.h[:, :, ti * 128:(ti + 1) * 128].rearrange("k p t -> p k t"), kt_[:], [kt_d])
                    if own:
                        o0 = (su - 1) * 512 + sb_ * 128
                        qt_ = qtt[ti % 2]
                        for pc in range(4):
                            S.tr(pt_[:, 256 + pc * 128:256 + (pc + 1) * 128], r_[:, 2 * pc:2 * pc + 2, :].rearrange("p a b -> p (a b)"), identb[:], [r_, identb], [pt_])
                        S.cp("act", qt_[:], pt_[:, 256:768].rearrange("p (a b) -> p a b", b=128), [pt_], [qt_])
                        S.st(qt_, qt_d.h.rearrange("p (a t) -> p a t", a=4)[:, :, o0:o0 + 128], qt_[:], [qt_d])
            if debug == 1:
                dt_ = S.sbuf("dt_", [128, 1024], F32)
                S.op("pool", lambda e: e.memset(dt_[:], 0.0), (), [dt_])
                S.cp("dve", dt_[:, 0:96], modT[:].rearrange("p a b -> p (a b)"), [modT], [dt_])
                kb_ = S.sbuf("kb_", [128, 128], BF16); qb_ = S.sbuf("qb_", [128, 128], BF16); vb_ = S.sbuf("vb_", [128, 132], BF16); k1_ = S.sbuf("k1_", [128, 128], BF16)
                S.ld(kb_, kb_[:], kt_d.h[0, :, 256:384], [kt_d]); S.ld(qb_, qb_[:], qt_d.h[:, 0:128], [qt_d])
                S.ld(vb_, vb_[:], va_d.h[:, 2 * 132:3 * 132], [va_d]); S.ld(k1_, k1_[:], kt_d.h[1, :, 0:128], [kt_d])
                S.op("dve", lambda e: e.tensor_copy(out=dt_[:, 128:256], in_=kb_[:]), [kb_], [dt_])
                S.op("dve", lambda e: e.tensor_copy(out=dt_[:, 256:384], in_=qb_[:]), [qb_], [dt_])
                S.op("dve", lambda e: e.tensor_copy(out=dt_[:, 384:516], in_=vb_[:]), [vb_], [dt_])
                S.op("dve", lambda e: e.tensor_copy(out=dt_[:, 640:768], in_=k1_[:]), [k1_], [dt_])
                S.st(dt_, dbg.h[:, :], dt_[:], [dbg])
            S.end_phase()
        if stop == 1:
            return nc

        with S.phase():
            NG = 32 if not (2 <= debug < 10) else 2
            lr = S.sbuf("lr", [64, 64], F32); li = S.sbuf("li", [64, 64], F32); lsp = S.sbuf("lsp", [64, 1], F32)
            S.ld(lr, lr[:], X.lam_re.h[:, :], [X.lam_re]); S.ld(li, li[:], X.lam_im.h[:, :], [X.lam_im]); S.ld(lsp, lsp[:], X.lstep.h[:, :], [X.lstep])
            rmask = S.sbuf("rmask", [128, 8], F32); S.ld(rmask, rmask[:], X.rmask_d.h[:, :], [X.rmask_d])
            dsk = S.sbuf("dsk", [128, 4], F32); S.ld(dsk, dsk[:], X.dskipT.h[:, :], [X.dskipT])
            sgn = S.sbuf("sgn", [128, 1], F32)
            S.op("dve", lambda e: e.memset(sgn[0:64, :], 1.0), (), [sgn]); S.op("dve", lambda e: e.memset(sgn[64:128, :], -1.0), (), [sgn])
            hpi = S.sbuf("hpi", [64, 1], F32); S.op("pool", lambda e: e.memset(hpi[:], float(np.pi / 2)), (), [hpi])
            zer = S.sbuf("zer", [64, 1], F32); S.op("pool", lambda e: e.memset(zer[:], 0.0), (), [zer])
            S.act(lsp[:], lsp[:], AF.Exp, [lsp], [lsp])
            ar = S.sbuf("ar", [64, 64], F32); th = S.sbuf("th", [64, 64], F32)
            S.ts("dve", ar[:], lr[:], lsp[:, 0:1], None, ALU.mult, None, [lr, lsp], [ar])
            S.ts("dve", th[:], li[:], lsp[:, 0:1], None, ALU.mult, None, [li, lsp], [th])
            rr = S.sbuf("rr", [64, 128], F32)
            S.act(rr[:, 0:64], ar[:], AF.Exp, [ar], [rr])
            CK = S.sbuf("CK", [64, 14, 128], F32); SK = S.sbuf("SK", [64, 14, 128], F32)
            cc = S.sbuf("cc", [64, 64], F32); s2_ = S.sbuf("s2_", [64, 64], F32)
            c_ = S.sbuf("c_", [64, 64], F32); s_ = S.sbuf("s_", [64, 64], F32)
            S.act(c_[:], th[:], AF.Sin, [th, hpi], [c_], bias=hpi[:, 0:1], scale=1.0 / 32)
            S.act(s_[:], th[:], AF.Sin, [th, zer], [s_], bias=zer[:, 0:1], scale=1.0 / 32)
            def dbl(co, so, ci, si):
                S.tt("dve", cc[:], ci, ci, ALU.mult, [c_, CK], [cc])
                S.tt("dve", s2_[:], si, si, ALU.mult, [s_, SK], [s2_])
                S.stt(so, ci, 2.0, si, ALU.mult, ALU.mult, [c_, s_, CK, SK], [s_, SK])
                S.tt("dve", co, cc[:], s2_[:], ALU.subtract, [cc, s2_], [c_, CK])
            for _ in range(4):
                dbl(c_[:], s_[:], c_[:], s_[:])
            dbl(CK[:, 0, 0:64], SK[:, 0, 0:64], c_[:], s_[:])
            for k in range(1, 14):
                dbl(CK[:, k, 0:64], SK[:, k, 0:64], CK[:, k - 1, 0:64], SK[:, k - 1, 0:64])
            S.cp("pool", CK[:, :, 64:128], CK[:, :, 0:64], [CK], [CK]); S.cp("pool", SK[:, :, 64:128], SK[:, :, 0:64], [SK], [SK])
            S.cp("pool", rr[:, 64:128], rr[:, 0:64], [rr], [rr])
            Lr = S.sbuf("Lr", [64, 64], F32); Li = S.sbuf("Li", [64, 64], F32)
            S.tt("dve", Lr[:], rr[:, 0:64], CK[:, 0, 0:64], ALU.mult, [rr, CK], [Lr])
            S.tt("dve", Li[:], rr[:, 0:64], SK[:, 0, 0:64], ALU.mult, [rr, SK], [Li])
            S.ts("dve", Lr[:], Lr[:], -1.0, None, ALU.add, None, [Lr], [Lr])
            den = S.sbuf("den", [64, 64], F32); tq = S.sbuf("tq", [64, 64], F32)
            S.tt("dve", den[:], lr[:], lr[:], ALU.mult, [lr], [den]); S.tt("dve", tq[:], li[:], li[:], ALU.mult, [li], [tq])
            S.tt("dve", den[:], den[:], tq[:], ALU.add, [den, tq], [den])
            S.op("dve", lambda e: e.reciprocal(out=den[:], in_=den[:]), [den], [den])
            cfr = S.sbuf("cfr", [64, 64], F32); cfi = S.sbuf("cfi", [64, 64], F32)
            S.tt("dve", cfr[:], Lr[:], lr[:], ALU.mult, [Lr, lr], [cfr]); S.tt("dve", tq[:], Li[:], li[:], ALU.mult, [Li, li], [tq])
            S.tt("dve", cfr[:], cfr[:], tq[:], ALU.add, [cfr, tq], [cfr]); S.tt("dve", cfr[:], cfr[:], den[:], ALU.mult, [cfr, den], [cfr])
            S.tt("dve", cfi[:], Li[:], lr[:], ALU.mult, [Li, lr], [cfi]); S.tt("dve", tq[:], Lr[:], li[:], ALU.mult, [Lr, li], [tq])
            S.tt("dve", cfi[:], cfi[:], tq[:], ALU.subtract, [cfi, tq], [cfi]); S.tt("dve", cfi[:], cfi[:], den[:], ALU.mult, [cfi, den], [cfi])
            PCs = S.sbuf("PCs", [128, 14, 64], F32); PSs = S.sbuf("PSs", [128, 14, 64], F32); Rs = S.sbuf("Rs", [128, 64], F32)
            ptr = [S.psum("ptr", [128, 512], F32) for _ in range(2)]
            for k in range(14):
                for w, (src, dst) in enumerate(((CK, PCs), (SK, PSs))):
                    p = ptr[(2 * k + w) % 2]
                    S.tr(p[:, 0:64], src[:, k, :], ident[0:64, 0:64], [src, ident], [p])
                    S.cp("act" if w else "dve", dst[:, k, :], p[:, 0:64], [p], [dst])
            S.tr(ptr[0][:, 0:64], rr[:], ident[0:64, 0:64], [rr, ident], [ptr[0]])
            S.cp("dve", Rs[:], ptr[0][:, 0:64], [ptr[0]], [Rs])
            Bst = S.sbuf("Bst", [128, 16, 128], BF16); Bsw = S.sbuf("Bsw", [128, 16, 128], BF16)
            C1 = S.sbuf("C1", [128, 16, 128], BF16); C2 = S.sbuf("C2", [128, 16, 128], BF16)
            S.op("pool", lambda e: e.memset(C1[:], 0.0), (), [C1]); S.op("pool", lambda e: e.memset(C2[:], 0.0), (), [C2])
            rp = S.sbuf("rp", [64, 128], F32); btr = S.sbuf("btr", [128, 64], F32); bti = S.sbuf("bti", [128, 64], F32)
            crr = S.sbuf("crr", [128, 64], F32); cri = S.sbuf("cri", [128, 64], F32)
            F1 = S.sbuf("F1", [128, 128], F32); F2 = S.sbuf("F2", [128, 128], F32); tb = S.sbuf("tb", [128, 64], F32)
            ca = S.sbuf("ca", [128, 128], F32); cb2 = S.sbuf("cb2", [128, 128], F32)

            def build_mats(ct):
                for d_ in range(2):
                    dct = d_ * 4 + ct
                    S.ld(rp, rp[:], X.rep_d.h[dct, :, :], [X.rep_d]); S.ld(btr, btr[:], X.bT_re.h[dct, :, :], [X.bT_re]); S.ld(bti, bti[:], X.bT_im.h[dct, :, :], [X.bT_im])
                    S.ld(ca, ca[:], X.cst_a.h[dct, :, :], [X.cst_a]); S.ld(cb2, cb2[:], X.cst_b.h[dct, :, :], [X.cst_b])
                    p = ptr[dct % 2]
                    S.mm(p[:, 0:64], rp[:], cfr[:], True, True, [rp, cfr], [p]); S.mm(p[:, 64:128], rp[:], cfi[:], True, True, [rp, cfi], [p])
                    S.cp("act", crr[:], p[:, 0:64], [p], [crr]); S.cp("act", cri[:], p[:, 64:128], [p], [cri])
                    S.tt("dve", F1[:, 0:64], crr[:], btr[:], ALU.mult, [crr, btr], [F1]); S.tt("dve", tb[:], cri[:], bti[:], ALU.mult, [cri, bti], [tb])
                    S.tt("dve", F1[:, 0:64], F1[:, 0:64], tb[:], ALU.subtract, [F1, tb], [F1])
                    S.tt("dve", F1[:, 64:128], crr[:], bti[:], ALU.mult, [crr, bti], [F1]); S.tt("dve", tb[:], cri[:], btr[:], ALU.mult, [cri, btr], [tb])
                    S.tt("dve", F1[:, 64:128], F1[:, 64:128], tb[:], ALU.add, [F1, tb], [F1])
                    S.cp("pool", F2[:, 0:64], F1[:, 64:128], [F1], [F2]); S.ts("pool", F2[:, 64:128], F1[:, 0:64], -1.0, None, ALU.mult, None, [F1], [F2])
                    for gl in range(8):
                        li_ = d_ * 8 + gl
                        S.ts("dve", Bst[:, li_, :], F1[:], rmask[:, gl:gl + 1], None, ALU.mult, None, [F1, rmask], [Bst])
                        S.ts("pool", Bsw[:, li_, :], F2[:], rmask[:, gl:gl + 1], None, ALU.mult, None, [F2, rmask], [Bsw])
                        S.ts("dve", C1[:, li_, gl * 16:(gl + 1) * 16], ca[:, gl * 16:(gl + 1) * 16], sgn[:, 0:1], None, ALU.mult, None, [ca, sgn], [C1])
                        S.ts("pool", C2[:, li_, gl * 16:(gl + 1) * 16], cb2[:, gl * 16:(gl + 1) * 16], -1.0, None, ALU.mult, None, [cb2], [C2])
            Tc = S.sbuf("Tc", [128, NALL], F32); Ts = S.sbuf("Ts", [128, NALL], F32)
            tmpa = S.sbuf("tmpa", [128, 512], F32)
            uTt = S.sbuf("uTt", [128, NALL], BF16)
            Yacc = S.sbuf("Yacc", [128, NOWN], F32)
            Rt = S.sbuf("Rt", [128, 512], F32); ones = S.sbuf("ones", [128, 512], F32)
            S.op("pool", lambda e: e.memset(ones[:], 1.0), (), [ones])
            Ssb = [S.sbuf("Ssb", [128, 512], F32) for _ in range(2)]; Wsb = [S.sbuf("Wsb", [128, 512], F32) for _ in range(2)]
            Ht = [S.sbuf("Ht", [128, 512], F32) for _ in range(2)]
            Hc = [S.sbuf("Hc", [128, 512], BF16) for _ in range(1)]; Hs = [S.sbuf("Hs", [128, 512], BF16) for _ in range(1)]
            pS = [S.psum("pS", [128, 512], F32) for _ in range(2)]; pW = [S.psum("pW", [128, 512], F32) for _ in range(2)]
            pY = [S.psum("pY", [128, 512], F32) for _ in range(2)]
            it = 0
            for ct in range(4):
                if ct * 8 >= NG:
                    S.op("pool", lambda e: e.memset(Yacc[:], 0.0), (), [Yacc])
                    S.st(Yacc, yT_d.h[ct * 128:(ct + 1) * 128, :], Yacc[:], [yT_d])
                    continue
                build_mats(ct)
                S.ld(uTt, uTt[:], uT_d.h[ct * 128:(ct + 1) * 128, :], [uT_d])
                S.ts("dve", Yacc[:], uTt[:, LC:LC + NOWN], dsk[:, ct:ct + 1], None, ALU.mult, None, [uTt, dsk], [Yacc])
                for gl in range(8):
                    for d_ in range(2):
                        S.maybe_sync()
                        gi = d_ * 32 + ct * 8 + gl
                        li_ = d_ * 8 + gl
                        TL = LC + NOWN if d_ == 0 else NALL
                        S.op("pool", lambda e: e.memset(Tc[:, 0:1], 1.0), (), [Tc]); S.op("pool", lambda e: e.memset(Ts[:, 0:1], 0.0), (), [Ts])
                        m = 1; k = 0
                        while m < TL:
                            nn = min(m, TL - m)
                            ck = PCs[:, k, gi:gi + 1]; sk = PSs[:, k, gi:gi + 1]
                            for o in range(0, nn, 512):
                                n = min(512, nn - o)
                                S.ts("pool", tmpa[:, 0:n], Ts[:, o:o + n], sk, None, ALU.mult, None, [Ts, PSs], [tmpa])
                                S.stt(Tc[:, m + o:m + o + n], Tc[:, o:o + n], ck, tmpa[:, 0:n], ALU.mult, ALU.subtract, [Tc, PCs, tmpa], [Tc])
                                S.ts("pool", tmpa[:, 0:n], Tc[:, o:o + n], sk, None, ALU.mult, None, [Tc, PSs], [tmpa])
                                S.stt(Ts[:, m + o:m + o + n], Ts[:, o:o + n], ck, tmpa[:, 0:n], ALU.mult, ALU.add, [Ts, PCs, tmpa], [Ts])
                            m *= 2; k += 1
                        S.ts("pool", Rt[:], ones[:], Rs[:, gi:gi + 1], None, ALU.mult, None, [ones, Rs], [Rt])
                        if d_ == 0:
                            chunks = [(0, LC, 0, False)] + [(LC + 512 * i, 512, LC + 512 * i, False) for i in range(8)]
                        else:
                            chunks = [(0, LC, LC - 1, True)] + [(LC + 512 * i, 512, NALL + LC - 1 - (LC + 512 * i), True) for i in range(15, -1, -1)]
                        prev = None
                        for (lo, n, tau, rev) in chunks:
                            ps_, pw_ = pS[it % 2], pW[it % 2]
                            ss_, ws_, h_ = Ssb[it % 2], Wsb[it % 2], Ht[it % 2]
                            x_ = ss_
                            S.mm(ps_[:, 0:n], Bst[:, li_, :], uTt[:, lo:lo + n], True, True, [Bst, uTt], [ps_])
                            S.mm(pw_[:, 0:n], Bsw[:, li_, :], uTt[:, lo:lo + n], True, True, [Bsw, uTt], [pw_])
                            S.cp("act", ss_[:, 0:n], ps_[:, 0:n], [ps_], [ss_]); S.cp("act", ws_[:, 0:n], pw_[:, 0:n], [pw_], [ws_])
                            if rev:
                                tcs = Tc[:, tau - n + 1:tau + 1][:, ::-1]; tss = Ts[:, tau - n + 1:tau + 1][:, ::-1]
                            else:
                                tcs = Tc[:, tau:tau + n]; tss = Ts[:, tau:tau + n]
                            me = "dve" if rev else "pool"
                            S.tt(me, ss_[:, 0:n], ss_[:, 0:n], tcs, ALU.mult, [ss_, Tc], [ss_])
                            S.tt(me, ws_[:, 0:n], ws_[:, 0:n], tss, ALU.mult, [ws_, Ts], [ws_])
                            S.tt("pool" if rev else "dve", x_[:, 0:n], ss_[:, 0:n], ws_[:, 0:n], ALU.add, [ss_, ws_], [x_])
                            if prev is None:
                                init = 0.0; rd = [Rt, x_]
                            else:
                                ph_, pn = prev
                                init = ph_[:, 0:1] if rev else ph_[:, pn - 1:pn]; rd = [Rt, x_, ph_]
                            if rev:
                                S.op("dve", lambda e, o=h_[:, 0:n][:, ::-1], a=Rt[:, 0:n], b=x_[:, 0:n][:, ::-1], i=init: e.tensor_tensor_scan(out=o, data0=a, data1=b, initial=i, op0=ALU.mult, op1=ALU.add), rd, [h_])
                            else:
                                S.op("dve", lambda e, o=h_[:, 0:n], a=Rt[:, 0:n], b=x_[:, 0:n], i=init: e.tensor_tensor_scan(out=o, data0=a, data1=b, initial=i, op0=ALU.mult, op1=ALU.add), rd, [h_])
                            prev = (h_, n)
                            if LC <= lo < LC + NOWN:
                                hc_, hs_, py_ = Hc[0], Hs[0], pY[it % 2]
                                S.tt(me, hc_[:, 0:n], h_[:, 0:n], tcs, ALU.mult, [h_, Tc], [hc_])
                                S.tt(me, hs_[:, 0:n], h_[:, 0:n], tss, ALU.mult, [h_, Ts], [hs_])
                                S.mm(py_[:, 0:n], C1[:, li_, :], hc_[:, 0:n], True, False, [C1, hc_], [py_])
                                S.mm(py_[:, 0:n], C2[:, li_, :], hs_[:, 0:n], False, True, [C2, hs_], [py_])
                                S.tt("dve", Yacc[:, lo - LC:lo - LC + n], Yacc[:, lo - LC:lo - LC + n], py_[:, 0:n], ALU.add, [Yacc, py_], [Yacc])
                            it += 1
                S.st(Yacc, yT_d.h[ct * 128:(ct + 1) * 128, :], Yacc[:], [yT_d])
            S.end_phase()
        with S.phase():
            NQB = 8 if not (2 <= debug < 10) else 1
            KT2 = [S.sbuf("KT2", [128, NALL], BF16) for _ in range(2)]
            VA = S.sbuf("VA", [128, 66, 2, 66], BF16)
            QT = S.sbuf("QT", [128, 4, NOWN], BF16)
            for kv in range(2):
                S.ld(KT2[kv], KT2[kv][:], kt_d.h[kv, :, :], [kt_d])
            S.ld(VA, VA[:].rearrange("p a b c -> p (a b c)"), va_d.h[:, :], [va_d])
            S.ld(QT, QT[:].rearrange("p a b -> p (a b)"), qt_d.h[:, :], [qt_d])
            pS = [S.psum("pS", [128, 512], F32) for _ in range(3)]
            pO = [S.psum("pO", [128, 4, 128], F32) for _ in range(2)]
            pR = S.psum("pR", [128, 1024], BF16)
            PT = [S.sbuf("PT", [128, 512], BF16) for _ in range(3)]
            otok = [S.sbuf("otok", [128, 4, 512], BF16) for _ in range(2)]
            oTs = [S.sbuf("oTs", [128, 4, 512], BF16) for _ in range(2)]
            rec = [S.sbuf("rec", [128, 4], F32) for _ in range(2)]
            it = 0
            for qb in range(NQB):
                ot = otok[qb % 2]
                for h in range(8):
                    S.maybe_sync()
                    kv, pc, base = h // 4, h // 2, 64 * (h % 2)
                    po = pO[h % 2]
                    for kt in range(66):
                        ps_, pt_ = pS[it % 3], PT[it % 3]
                        S.mm(ps_[:], KT2[kv][base:base + 64, kt * 128:(kt + 1) * 128], QT[base:base + 64, pc, qb * 512:(qb + 1) * 512], True, True, [KT2[kv], QT], [ps_])
                        S.act(pt_[:], ps_[:], AF.Exp, [ps_], [pt_])
                        for sub in range(4):
                            S.mm(po[:, sub, 0:65], pt_[:, sub * 128:(sub + 1) * 128], VA[:, kt, kv, 0:65], kt == 0 and sub == 0, kt == 65 and sub == 3, [pt_, VA], [po])
                        it += 1
                    rc_ = rec[h % 2]
                    S.op("dve", lambda e, rc_=rc_, po=po: e.reciprocal(out=rc_[:], in_=po[:, :, 64]), [po], [rc_])
                    S.tt("dve", ot[:, :, h * 64:(h + 1) * 64], po[:, :, 0:64], rc_[:].unsqueeze(2).to_broadcast([128, 4, 64]), ALU.mult, [po, rc_], [ot])
                o2 = oTs[qb % 2]
                for sub in range(4):
                    for c in range(4):
                        S.tr(pR[:, c * 128:(c + 1) * 128], ot[:, sub, c * 128:(c + 1) * 128], identb[:], [ot, identb], [pR])
                    S.cp("act" if sub % 2 else "dve", o2[:, :, sub * 128:(sub + 1) * 128], pR[:, 0:512].rearrange("p (a b) -> p a b", b=128), [pR], [o2])
                S.st(o2, oT_d.h[:, qb * 512:(qb + 1) * 512].rearrange("(m p) t -> p m t", p=128), o2[:], [oT_d])
            S.end_phase()
        with S.phase():
            NST = 8 if not (2 <= debug < 10) else 1
            stg = S.sbuf("stg", [128, 8, 512], F32)
            wgl = S.sbuf("wgl", [128, 4, 512], BF16); wss = S.sbuf("wss", [128, 4, D], BF16)
            wat = S.sbuf("wat", [128, 4, D], BF16); wou = S.sbuf("wou", [128, 8, D], BF16)
            S.ld(stg, stg[:, 0:4, :], X.w_glu.h.rearrange("(k p) c -> p k c", p=128), [X.w_glu]); S.cp("act", wgl[:], stg[:, 0:4, :], [stg], [wgl])
            for hf in range(2):
                S.ld(stg, stg[:, 0:4, :], X.w_ssm.h[:, hf * 512:(hf + 1) * 512].rearrange("(k p) c -> p k c", p=128), [X.w_ssm]); S.cp("pool", wss[:, :, hf * 512:(hf + 1) * 512], stg[:, 0:4, :], [stg], [wss])
                S.ld(stg, stg[:, 0:4, :], X.w_att.h[:, hf * 512:(hf + 1) * 512].rearrange("(k p) c -> p k c", p=128), [X.w_att]); S.cp("act", wat[:, :, hf * 512:(hf + 1) * 512], stg[:, 0:4, :], [stg], [wat])
                S.ld(stg, stg[:], X.w_out.h[:, hf * 512:(hf + 1) * 512].rearrange("(k p) c -> p k c", p=128), [X.w_out]); S.cp("pool", wou[:, :, hf * 512:(hf + 1) * 512], stg[:], [stg], [wou])
            rwt = S.sbuf("rwt", [128, 8, 32], F32); S.ld(rwt, rwt[:], X.rw_d.h.rearrange("(k p) c -> p k c", p=128), [X.rw_d])
            rbt = S.sbuf("rbt", [128, 32], F32); S.ld(rbt, rbt[:], _bc(X.rb_d.h[0:1, :]), [X.rb_d])
            bgl = S.sbuf("bgl", [128, 4], F32); S.ld(bgl, bgl[:], X.b_gluT.h[:, :], [X.b_gluT])
            g1r = S.sbuf("g1r", [128, D], F32)
            S.ld(g1r, g1r[:], _bc(modrow_d.h[0:8, :].rearrange("a b -> (a b)").unsqueeze(0)), [modrow_d])
            yt = S.sbuf("yt", [128, 4, 512], F32); ot_ = S.sbuf("ot_", [128, 4, 512], BF16); gst = S.sbuf("gst", [128, 16, 512], BF16)
            x2 = S.sbuf("x2", [128, 4, 512], F32); sT = S.sbuf("sT", [128, 4, 512], BF16); s2T = S.sbuf("s2T", [128, 4, 512], BF16)
            sig = S.sbuf("sig", [128, 512], F32); mT = S.sbuf("mT", [128, 8, 512], BF16)
            ta = S.sbuf("ta", [128, 512], F32); tb2 = S.sbuf("tb2", [128, 512], F32)
            xt = [S.sbuf("xt", [128, D], F32) for _ in range(2)]; xm = [S.sbuf("xm", [128, D], F32) for _ in range(2)]
            sq = S.sbuf("sq", [128, D], BF16); ss = [S.sbuf("ss", [128, 1], F32) for _ in range(2)]
            xn = S.sbuf("xn", [128, D], F32); h2f = S.sbuf("h2f", [128, 8, 128], F32); h2b = [S.sbuf("h2b", [128, 8, 128], BF16) for _ in range(2)]
            lg = S.sbuf("lg", [128, 32], F32); t8 = S.sbuf("t8", [128, 8], F32); msk = S.sbuf("msk", [128, 32], F32)
            ex = S.sbuf("ex", [128, 32], F32); sm = S.sbuf("sm", [128, 1], F32); gsb = S.sbuf("gsb", [32, 128], F32)
            pA = [S.psum("pA", [128, 512], F32) for _ in range(2)]; pB = [S.psum("pB", [128, 512], F32) for _ in range(2)]
            pX = [S.psum("pX", [128, 512], F32) for _ in range(2)]; pL = S.psum("pL", [128, 512], F32)
            for st_ in range(NST):
                S.maybe_sync()
                c0 = st_ * 512
                S.ld(yt, yt[:], yT_d.h[:, c0:c0 + 512].rearrange("(m p) t -> p m t", p=128), [yT_d])
                S.ld(ot_, ot_[:], oT_d.h[:, c0:c0 + 512].rearrange("(m p) t -> p m t", p=128), [oT_d])
                S.ld(gst, gst[:], gs_d.h[:, c0:c0 + 512].rearrange("(m p) t -> p m t", p=128), [gs_d])
                S.tt("pool", x2[:], yt[:], yt[:], ALU.mult, [yt], [x2])
                S.ts("dve", x2[:], x2[:], 0.044715, 1.0, ALU.mult, ALU.add, [x2], [x2])
                S.tt("pool", x2[:], x2[:], yt[:], ALU.mult, [x2, yt], [x2])
                S.act(x2[:], x2[:], AF.Sigmoid, [x2], [x2], scale=1.5957691216057308)
                S.tt("dve", sT[:], x2[:], yt[:], ALU.mult, [x2, yt], [sT])
                for mo in range(4):
                    p = pA[mo % 2]
                    for kc in range(4):
                        S.mm(p[:], wgl[:, kc, mo * 128:(mo + 1) * 128], sT[:, kc, :], kc == 0, kc == 3, [wgl, sT], [p])
                    S.act(sig[:], p[:], AF.Sigmoid, [p, bgl], [sig], bias=bgl[:, mo:mo + 1])
                    S.tt("dve", s2T[:, mo, :], sT[:, mo, :], sig[:], ALU.mult, [sT, sig], [s2T])
                for mo in range(8):
                    pa, pb = pA[mo % 2], pB[mo % 2]
                    for kc in range(4):
                        S.mm(pa[:], wss[:, kc, mo * 128:(mo + 1) * 128], s2T[:, kc, :], kc == 0, kc == 3, [wss, s2T], [pa])
                    for kc in range(4):
                        S.mm(pb[:], wat[:, kc, mo * 128:(mo + 1) * 128], ot_[:, kc, :], kc == 0, kc == 3, [wat, ot_], [pb])
                    S.tt("dve", ta[:], pa[:], gst[:, mo, :], ALU.mult, [pa, gst], [ta])
                    S.tt("dve", tb2[:], pb[:], gst[:, 8 + mo, :], ALU.mult, [pb, gst], [tb2])
                    S.tt("pool", mT[:, mo, :], ta[:], tb2[:], ALU.add, [ta, tb2], [mT])
                for sub in range(4):
                    r0 = c0 + sub * 128
                    x_, m_, s_ = xt[sub % 2], xm[sub % 2], ss[sub % 2]
                    S.ld(x_, x_[:], X.xs.h[r0:r0 + 128, :], [X.xs])
                    for hf in range(2):
                        p = pX[hf]
                        for kc in range(8):
                            S.mm(p[:], mT[:, kc, sub * 128:(sub + 1) * 128], wou[:, kc, hf * 512:(hf + 1) * 512], kc == 0, kc == 7, [mT, wou], [p])
                        S.tt("dve", m_[:, hf * 512:(hf + 1) * 512], p[:], g1r[:, hf * 512:(hf + 1) * 512], ALU.mult, [p, g1r], [m_])
                    S.tt("pool", m_[:], m_[:], x_[:], ALU.add, [m_, x_], [m_])
                    S.st(m_, xmix_d.h[r0:r0 + 128, :], m_[:], [xmix_d])
                    S.act(sq[:], m_[:], AF.Square, [m_], [sq, s_], accum=s_[:])
                    S.ts("dve", s_[:], s_[:], 1.0 / D, EPS, ALU.mult, ALU.add, [s_], [s_])
                    S.act(s_[:], s_[:], AF.Sqrt, [s_], [s_])
                    S.op("dve", lambda e, s_=s_: e.reciprocal(out=s_[:], in_=s_[:]), [s_], [s_])
                    S.ts("dve", xn[:], m_[:], s_[:, 0:1], None, ALU.mult, None, [m_, s_], [xn])
                    hb = h2b[sub % 2]
                    for kc in range(8):
                        p = pX[kc % 2]
                        S.tr(p[:, 0:128], xn[:, kc * 128:(kc + 1) * 128], ident[:], [xn, ident], [p])
                        S.ts("dve", h2f[:, kc, :], p[:, 0:128], G2[:, kc:kc + 1], modT[:, 24 + kc, 0:1], ALU.mult, ALU.add, [p, G2, modT], [h2f])
                    S.cp("pool", hb[:], h2f[:], [h2f], [hb])
                    S.st(hb, h2T_d.h[:, r0:r0 + 128].rearrange("(m p) t -> p m t", p=128), hb[:], [h2T_d])
                    for kc in range(8):
                        S.mm(pL[:, 0:32], h2f[:, kc, :], rwt[:, kc, :], kc == 0, kc == 7, [h2f, rwt], [pL])
                    S.tt("dve", lg[:], pL[:, 0:32], rbt[:], ALU.add, [pL, rbt], [lg])
                    S.op("dve", lambda e: e.max(out=t8[:], in_=lg[:]), [lg], [t8])
                    S.ts("dve", msk[:], lg[:], t8[:, 3:4], None, ALU.is_ge, None, [lg, t8], [msk])
                    S.ts("dve", sm[:], t8[:, 0:1], -1.0, None, ALU.mult, None, [t8], [sm])
                    S.act(ex[:], lg[:], AF.Exp, [lg, sm], [ex], bias=sm[:, 0:1])
                    S.tt("dve", ex[:], ex[:], msk[:], ALU.mult, [ex, msk], [ex])
                    S.op("dve", lambda e: e.tensor_reduce(out=sm[:], in_=ex[:], axis=AX.X, op=ALU.add), [ex], [sm])
                    S.op("dve", lambda e: e.reciprocal(out=sm[:], in_=sm[:]), [sm], [sm])
                    S.ts("dve", ex[:], ex[:], sm[:, 0:1], None, ALU.mult, None, [ex, sm], [ex])
                    S.tr(pL[0:32, 128:256], ex[:], ident[:], [ex, ident], [pL])
                    S.cp("dve", gsb[:], pL[0:32, 128:256], [pL], [gsb])
                    S.st(gsb, gT_d.h[:, r0:r0 + 128], gsb[:], [gT_d])
            if debug == 4:
                S.ld(xn, xn[:], xmix_d.h[0:128, :], [xmix_d])
                S.st(xn, dbg.h[:, :], xn[:], [dbg])
            S.end_phase()
        if stop == 4:
            return nc
        with S.phase():
            dbgm = 2 <= debug < 10
            NTH, NSTT = (1, 1) if dbgm else (4, 2)
            h2 = S.sbuf("h2", [128, 8, 1024], BF16)
            acc = S.sbuf("acc", [128, 8, D], F32)
            Wg = [S.sbuf("Wg", [128, 8, 256], BF16) for _ in range(8)]
            Wdn = [S.sbuf("Wdn", [128, D], BF16) for _ in range(8)]
            sg_ = [S.sbuf("sgst", [128, 8, 128], F32) for _ in range(2)]
            sd_ = [S.sbuf("sdst", [128, D], F32) for _ in range(2)]
            aT = [S.sbuf("aT", [128, 8, 512], BF16) for _ in range(2)]
            bgt = [S.sbuf("bgt", [128, 16], F32) for _ in range(2)]
            Ge = [S.sbuf("Ge", [128, 512], F32) for _ in range(2)]
            gB = [S.sbuf("g_", [128, 512], F32) for _ in range(2)]; uB = [S.sbuf("u_", [128, 512], F32) for _ in range(2)]
            sB = [S.sbuf("sgm", [128, 512], F32) for _ in range(2)]
            pG = [S.psum("pG", [128, 512], F32) for _ in range(2)]; pU = [S.psum("pU", [128, 512], F32) for _ in range(2)]
            pD = [S.psum("pD", [128, 512], F32) for _ in range(2)]
            g2r = S.sbuf("g2r", [128, D], F32); fgr = S.sbuf("fgr", [128, D], F32)
            S.ld(g2r, g2r[:], _bc(modrow_d.h[8:16, :].rearrange("a b -> (a b)").unsqueeze(0)), [modrow_d])
            S.ld(fgr, fgr[:], _bc(X.fg_d.h[0:1, :]), [X.fg_d])
            bds = S.sbuf("bds", [32, D], F32); S.ld(bds, bds[:], X.bdn.h[:, :], [X.bdn])
            gtt = S.sbuf("gtt", [32, 128], F32)
            ssf = S.sbuf("ssf", [128, 1], F32); sqf = S.sbuf("sqf", [128, D], BF16)
            ldc = [0]

            def load_g(e, j):
                for hf_ in range(2):
                    stg_ = sg_[ldc[0] % 2]; ldc[0] += 1
                    c_ = hf_ * 1024 + j * 128
                    S.ld(stg_, stg_[:], WGU(e, 0, D, c_, c_ + 128).rearrange("(k p) c -> p k c", p=128), [wgu_all if gather else X.wgu_d])
                    S.cp("act" if hf_ else "pool", Wg[j][:, :, hf_ * 128:(hf_ + 1) * 128], stg_[:], [stg_], [Wg[j]])

            def load_d(e, j):
                stg_ = sd_[ldc[0] % 2]; ldc[0] += 1
                S.ld(stg_, stg_[:], WD(e, j * 128, (j + 1) * 128, 0, D), [wd_all if gather else X.wd_d])
                S.cp("pool" if j % 2 else "act", Wdn[j][:], stg_[:], [stg_], [Wdn[j]])

            for th in range(NTH):
                t0 = th * 1024
                nld = NSTT * 512
                S.ld(h2, h2[:, :, 0:nld], h2T_d.h[:, t0:t0 + nld].rearrange("(m p) t -> p m t", p=128), [h2T_d])
                for j in range(8):
                    load_g(0, j)
                for j in range(8):
                    load_d(0, j)
                for e in range(32):
                    S.maybe_sync()
                    bg = bgt[e % 2]
                    S.ld(bg, bg[:], X.bguT.h[e, :, :], [X.bguT])
                    for st_ in range(NSTT):
                        c0 = st_ * 512
                        ge = Ge[(e * NSTT + st_) % 2]
                        S.ld(ge, ge[:], _bc(gT_d.h[e:e + 1, t0 + c0:t0 + c0 + 512]), [gT_d])
                        a_ = aT[(e * NSTT + st_) % 2]
                        for j in range(8):
                            pg, pu = pG[j % 2], pU[j % 2]
                            g_, u_, sgm = gB[j % 2], uB[j % 2], sB[j % 2]
                            for kc in range(8):
                                S.mm(pg[:], Wg[j][:, kc, 0:128], h2[:, kc, c0:c0 + 512], kc == 0, kc == 7, [Wg[j], h2], [pg])
                            for kc in range(8):
                                S.mm(pu[:], Wg[j][:, kc, 128:256], h2[:, kc, c0:c0 + 512], kc == 0, kc == 7, [Wg[j], h2], [pu])
                            S.ts("dve", g_[:], pg[:], bg[:, j:j + 1], 7.0, ALU.add, ALU.min, [pg, bg], [g_])
                            S.act(sgm[:], g_[:], AF.Sigmoid, [g_], [sgm], scale=1.702)
                            S.ts("dve", u_[:], pu[:], bg[:, 8 + j:9 + j], 7.0, ALU.add, ALU.min, [pu, bg], [u_])
                            S.ts("pool", u_[:], u_[:], -7.0, 1.0, ALU.max, ALU.add, [u_], [u_])
                            S.tt("pool", g_[:], g_[:], sgm[:], ALU.mult, [g_, sgm], [g_])
                            S.tt("pool", g_[:], g_[:], u_[:], ALU.mult, [g_, u_], [g_])
                            S.tt("dve", a_[:, j, :], g_[:], ge[:], ALU.mult, [g_, ge], [a_])
                            if st_ == NSTT - 1 and e + 1 < 32:
                                load_g(e + 1, j)
                        for sub in range(4):
                            for hf in range(2):
                                pd = pD[(sub * 2 + hf) % 2]
                                for j in range(8):
                                    S.mm(pd[:], a_[:, j, sub * 128:(sub + 1) * 128], Wdn[j][:, hf * 512:(hf + 1) * 512], j == 0, j == 7, [a_, Wdn[j]], [pd])
                                av = acc[:, st_ * 4 + sub, hf * 512:(hf + 1) * 512]
                                if e == 0:
                                    S.cp("dve", av, pd[:], [pd], [acc])
                                else:
                                    S.tt("dve", av, av, pd[:], ALU.add, [acc, pd], [acc])
                    if e + 1 < 32:
                        for j in range(8):
                            load_d(e + 1, j)
                for ti in range(4 * NSTT):
                    r0 = t0 + ti * 128
                    S.ld(gtt, gtt[:], gT_d.h[:, r0:r0 + 128], [gT_d])
                    xm_ = sd_[ti % 2]
                    S.ld(xm_, xm_[:], xmix_d.h[r0:r0 + 128, :], [xmix_d])
                    for hf in range(2):
                        pd = pD[hf]
                        S.mm(pd[:], gtt[:], bds[:, hf * 512:(hf + 1) * 512], True, True, [gtt, bds], [pd])
                        av = acc[:, ti, hf * 512:(hf + 1) * 512]
                        S.tt("dve", av, av, pd[:], ALU.add, [acc, pd], [acc])
                    S.tt("pool", acc[:, ti, :], acc[:, ti, :], g2r[:], ALU.mult, [acc, g2r], [acc])
                    S.tt("dve", acc[:, ti, :], acc[:, ti, :], xm_[:], ALU.add, [acc, xm_], [acc])
                    S.act(sqf[:], acc[:, ti, :], AF.Square, [acc], [sqf, ssf], accum=ssf[:])
                    S.ts("dve", ssf[:], ssf[:], 1.0 / D, EPS, ALU.mult, ALU.add, [ssf], [ssf])
                    S.act(ssf[:], ssf[:], AF.Sqrt, [ssf], [ssf])
                    S.op("dve", lambda en: en.reciprocal(out=ssf[:], in_=ssf[:]), [ssf], [ssf])
                    S.stt(xm_[:], acc[:, ti, :], ssf[:, 0:1], fgr[:], ALU.mult, ALU.mult, [acc, ssf, fgr], [xm_])
                    S.st(xm_, out_d.h[r0:r0 + 128, :], xm_[:], [out_d])
            S.end_phase()
    return nc


_NC_CACHE = {}


def _rope_tables(pos):
    half = 32
    inv = (10000.0 ** (-np.arange(0, half, 2, dtype=np.float32) / half)).astype(np.float32)
    row = (pos // 64).astype(np.float32)
    col = (pos % 64).astype(np.float32)
    ang = np.concatenate([row[:, None] * inv, col[:, None] * inv], axis=-1).astype(np.float32)
    return np.cos(ang).astype(np.float32), np.sin(ang).astype(np.float32)


def prep_inputs(inp, gather=True):
    f = lambda a: np.ascontiguousarray(np.asarray(a, dtype=np.float32))
    x, c, ctx, c_ctx = f(inp["x"]), f(inp["c"]), f(inp["ctx"]), f(inp["c_ctx"])
    colT = lambda v, n: f(np.asarray(v, np.float32).reshape(n, 128).T)
    shared = {
        "w_mod": f(inp["w_mod"][0]),
        "b_modT": colT(inp["b_mod"][0], 48),
        "n1T": colT(inp["norm1_g"][0], 8),
        "n2T": colT(inp["norm2_g"][0], 8),
        "w_in": f(inp["w_in"][0]),
        "ident": np.eye(128, dtype=np.float32),
        "qg": f(inp["q_norm_g"][0]).reshape(1, 64),
        "kg": f(inp["k_norm_g"][0]).reshape(1, 64),
        "rmask": f((np.arange(128)[:, None] // 16 == np.arange(8)[None, :])),
        "dskipT": colT(inp["s5_d"][0], 4),
        "w_glu": f(inp["w_glu"][0]), "b_gluT": colT(inp["b_glu"][0], 4),
        "w_ssm": f(inp["w_ssm_out"][0]), "w_att": f(inp["w_attn_out"][0]), "w_out": f(inp["w_out"][0]),
        "rw": f(inp["router_w"][0]), "rb": f(inp["router_b"][0]).reshape(1, 32),

        "bguT": f(np.asarray(inp["b_gate_up"][0], np.float32).reshape(32, 16, 128).transpose(0, 2, 1)),
        "bdn": f(inp["b_down"][0]), "fg": f(inp["final_norm_g"]).reshape(1, D),
    }
    rep = np.zeros((8, 64, 128), np.float32)
    for d_ in range(2):
        for ct in range(4):
            for gl in range(8):
                rep[d_ * 4 + ct, d_ * 32 + ct * 8 + gl, gl * 16:(gl + 1) * 16] = 1.0
    shared["rep"] = rep
    maps = []
    for core in range(8):
        b, half = core // 2, core % 2
        pos = np.arange(L)
        if half == 0:
            xs_, cs_ = x[b], ctx[b]
        else:
            xs_, cs_ = x[b][::-1], ctx[b][::-1]
            pos = pos[::-1]
        rc_, rs_ = _rope_tables(pos)
        m = dict(shared)
        do = [0, 1] if half == 0 else [1, 0]
        g5 = lambda k: np.asarray(inp[k][0], np.float32)[do]
        m["lam_re"] = f(g5("s5_lam_re").reshape(64, 64))
        m["lam_im"] = f(g5("s5_lam_im").reshape(64, 64))
        m["lstep"] = f(g5("s5_log_step").reshape(64, 1))
        tb_ = lambda a: f(a.reshape(2, 4, 8, 64, 16).transpose(0, 1, 2, 4, 3).reshape(8, 128, 64))
        m["bT_re"], m["bT_im"] = tb_(g5("s5_b_re")), tb_(g5("s5_b_im"))
        tc_ = lambda a: a.reshape(2, 4, 8, 16, 64).transpose(0, 1, 4, 2, 3).reshape(8, 64, 128)
        cr_, ci_ = tc_(g5("s5_c_re")), tc_(g5("s5_c_im"))
        m["cst_a"] = f(np.concatenate([cr_, ci_], axis=1))
        m["cst_b"] = f(np.concatenate([ci_, cr_], axis=1))
        if gather:
            m["wgu_sh"] = f(inp["w_gate_up"][0][4 * core:4 * core + 4]).reshape(4 * D, 2048)
            m["wd_sh"] = f(inp["w_down"][0][4 * core:4 * core + 4]).reshape(4 * D, D)
        else:
            m["wgu"], m["wd"] = f(inp["w_gate_up"][0]), f(inp["w_down"][0])
        m.update({"xs": f(xs_), "ctxs": f(cs_), "cvec": f(np.stack([c[b], c_ctx], axis=1).reshape(8, 128, 2).transpose(1, 0, 2)),
                  "ropec": f(rc_), "ropes": f(rs_)})
        maps.append(m)
    return maps


def kernel(**inputs):
    maps = prep_inputs(inputs, gather=False)
    if "nc" not in _NC_CACHE:
        _NC_CACHE["nc"] = build(debug=False, gather=False)
    res = run_bass_kernel_spmd(_NC_CACHE["nc"], maps, core_ids=list(range(8)))
    out = np.zeros((4, L, D), np.float32)
    for core in range(8):
        b, half = core // 2, core % 2
        y = np.asarray(res.results[core]["out"], np.float32)
        if half == 0:
            out[b, :NOWN] = y
        else:
            out[b, NOWN:] = y[::-1]
    return out
```
